# Optimizing a Trainium2 kernel written in Bass

```python
import jax, jax.numpy as jnp
from jax import lax
import numpy as np

D_MODEL = 2048
BATCH = 2
SEQ = 8192
DEPTH = 1

EPS = 1e-6
CHUNK = 64

A_HEADS = 8
A_DK = 128
A_DV = 128
A_WIDTH = A_HEADS * A_DV

B_QK_HEADS = 4
B_V_HEADS = 8
B_DK = 128
B_DV = 128
B_WIDTH = B_V_HEADS * B_DV
CONV = 4
B_CONV_CH = 2 * B_QK_HEADS * B_DK + B_WIDTH

MIX_WIDTH = A_WIDTH + B_WIDTH

IN_SIZES = (A_HEADS * A_DK, A_HEADS * A_DK, A_WIDTH, A_WIDTH,
            B_QK_HEADS * B_DK, B_QK_HEADS * B_DK, B_WIDTH, B_WIDTH,
            B_V_HEADS, B_V_HEADS)
IN_WIDTH = 4 * A_HEADS * A_DK + 2 * B_QK_HEADS * B_DK + 2 * B_WIDTH + 2 * B_V_HEADS

P_HEADS = 8
N_KEYS = 128
N_EXPERTS = N_KEYS * N_KEYS
P_DKEY = 256
P_TOPK = 16
P_BLOCK = 128

kernel_name = "hymba_hgrn2_gdn_peer"


def _rmsnorm(x, g):
    xf = x.astype(jnp.float32)
    y = xf * lax.rsqrt(jnp.mean(xf * xf, axis=-1, keepdims=True) + EPS)
    return (y * g.astype(jnp.float32)).astype(x.dtype)


def _gated_rmsnorm(o, z, g):
    of = o.astype(jnp.float32)
    y = of * lax.rsqrt(jnp.mean(of * of, axis=-1, keepdims=True) + EPS)
    return y * g.astype(jnp.float32) * jax.nn.silu(z.astype(jnp.float32))


def _l2norm(x):
    xf = x.astype(jnp.float32)
    return xf * lax.rsqrt(jnp.sum(xf * xf, axis=-1, keepdims=True) + EPS)


def _chunk(t):
    b, s, h = t.shape[:3]
    rest = t.shape[3:]
    t = t.reshape((b, s // CHUNK, CHUNK, h) + rest)
    return t.transpose((1, 0, 3, 2) + tuple(range(4, t.ndim)))


def _unchunk(t):
    n, b, h, c, d = t.shape
    return t.transpose(1, 0, 3, 2, 4).reshape(b, n * c, h, d)


def _causal_conv_silu(x, w):
    s = x.shape[1]
    xp = jnp.pad(x, ((0, 0), (CONV - 1, 0), (0, 0)))
    y = xp[:, 0:s, :] * w[0]
    for j in range(1, CONV):
        y = y + xp[:, j:j + s, :] * w[j]
    return jax.nn.silu(y)


def _hgrn2(q, f_logit, v, lb):
    q = q.astype(jnp.float32)
    z = f_logit.astype(jnp.float32)
    v = v.astype(jnp.float32)
    lb = lb.astype(jnp.float32)
    log_f = jnp.log(lb + (1.0 - lb) * jax.nn.sigmoid(z))
    k = (1.0 - lb) * jax.nn.sigmoid(-z)
    qc, kc, vc = _chunk(q), _chunk(k), _chunk(v)
    bc = jnp.cumsum(_chunk(log_f), axis=-2)
    causal = jnp.tril(jnp.ones((CHUNK, CHUNK), dtype=bool))

    def step(S, inp):
        q_c, k_c, v_c, b_c = inp
        diff = b_c[..., :, None, :] - b_c[..., None, :, :]
        decay = jnp.exp(jnp.where(causal[:, :, None], diff, -jnp.inf))
        att = jnp.einsum('bhtd,bhsd,bhtsd->bhts', q_c, k_c, decay)
        o = (jnp.einsum('bhtd,bhdv->bhtv', q_c * jnp.exp(b_c), S)
             + jnp.einsum('bhts,bhsv->bhtv', att, v_c))
        b_last = b_c[..., -1:, :]
        S = (S * jnp.exp(b_last[..., 0, :, None])
             + jnp.einsum('bhsd,bhsv->bhdv', k_c * jnp.exp(b_last - b_c), v_c))
        return S, o

    S0 = jnp.zeros((q.shape[0], q.shape[2], A_DK, A_DV), jnp.float32)
    _, o = lax.scan(step, S0, (qc, kc, vc, bc))
    return _unchunk(o)


def _gated_delta(q, k, v, beta, g):
    q = _chunk(q.astype(jnp.float32) * (B_DK ** -0.5))
    k = _chunk(k.astype(jnp.float32))
    v = _chunk(v.astype(jnp.float32))
    beta = _chunk(beta.astype(jnp.float32))
    gc = jnp.cumsum(_chunk(g.astype(jnp.float32)), axis=-1)
    incl = jnp.tril(jnp.ones((CHUNK, CHUNK), dtype=bool))
    strict = jnp.tril(jnp.ones((CHUNK, CHUNK), dtype=bool), k=-1)
    L = jnp.exp(jnp.where(incl, gc[..., :, None] - gc[..., None, :], -jnp.inf))
    kb = k * beta[..., None]
    M = jnp.eye(CHUNK, dtype=jnp.float32) + jnp.where(
        strict, jnp.einsum('...td,...sd->...ts', kb, k) * L, 0.0)
    u = lax.linalg.triangular_solve(M, v * beta[..., None], left_side=True,
                                    lower=True, unit_diagonal=True)
    w = lax.linalg.triangular_solve(M, kb * jnp.exp(gc)[..., None], left_side=True,
                                    lower=True, unit_diagonal=True)
    a_qk = jnp.einsum('...td,...sd->...ts', q, k) * L
    qg = q * jnp.exp(gc)[..., None]
    g_last = gc[..., -1:]
    kd = k * jnp.exp(g_last - gc)[..., None]
    dl = jnp.exp(g_last)[..., None]

    def step(S, inp):
        qg_c, kd_c, u_c, w_c, a_c, dl_c = inp
        v_new = u_c - jnp.einsum('bhtd,bhdv->bhtv', w_c, S)
        o = (jnp.einsum('bhtd,bhdv->bhtv', qg_c, S)
             + jnp.einsum('bhts,bhsv->bhtv', a_c, v_new))
        S = S * dl_c + jnp.einsum('bhsd,bhsv->bhdv', kd_c, v_new)
        return S, o

    S0 = jnp.zeros((qg.shape[1], qg.shape[2], B_DK, B_DV), jnp.float32)
    _, o = lax.scan(step, S0, (qg, kd, u, w, a_qk, dl))
    return _unchunk(o)


def _peer(xn, w_query, sub_keys, u_tab, v_tab):
    b, s, d = xn.shape
    t = b * s
    xt = xn.reshape(t, d)
    qry = jnp.einsum('td,dhk->thk', xt, w_query).astype(jnp.float32)
    half = P_DKEY // 2
    s1 = jnp.einsum('thk,hnk->thn', qry[..., :half], sub_keys[0].astype(jnp.float32))
    s2 = jnp.einsum('thk,hnk->thn', qry[..., half:], sub_keys[1].astype(jnp.float32))
    v1, i1 = lax.top_k(s1, P_TOPK)
    v2, i2 = lax.top_k(s2, P_TOPK)
    cand = (v1[..., :, None] + v2[..., None, :]).reshape(t, P_HEADS, P_TOPK * P_TOPK)
    best, pos = lax.top_k(cand, P_TOPK)
    expert = (jnp.take_along_axis(i1, pos // P_TOPK, axis=-1) * N_KEYS
              + jnp.take_along_axis(i2, pos % P_TOPK, axis=-1))
    gate = jax.nn.softmax(best, axis=-1)
    nblk = t // P_BLOCK

    def blk(args):
        xb, eb, gb = args
        hid = jax.nn.gelu(jnp.einsum('pd,phkd->phk', xb, u_tab[eb]).astype(jnp.float32),
                          approximate=False)
        return jnp.einsum('phk,phkd->pd', (gb * hid).astype(xb.dtype), v_tab[eb])

    y = lax.map(blk, (xt.reshape(nblk, P_BLOCK, d),
                      expert.reshape(nblk, P_BLOCK, P_HEADS, P_TOPK),
                      gate.reshape(nblk, P_BLOCK, P_HEADS, P_TOPK)))
    return y.reshape(b, s, d)


def setup_inputs(seed: int = 0) -> dict:
    key = jax.random.key(seed)
    ks = jax.random.split(key, 20)
    f32 = jnp.float32
    nrm = lambda k, shape, scale: jax.random.normal(k, shape, f32) * scale
    gain = lambda k, shape: 1.0 + 0.02 * jax.random.normal(k, shape, f32)
    dt = jnp.exp(jax.random.uniform(ks[8], (DEPTH, B_V_HEADS), f32,
                                    minval=np.log(1e-3), maxval=np.log(1e-1)))
    return {
        "x": nrm(ks[0], (BATCH, SEQ, D_MODEL), 1.0),
        "attn_norm_g": gain(ks[1], (DEPTH, D_MODEL)),
        "w_in": nrm(ks[2], (DEPTH, D_MODEL, IN_WIDTH), D_MODEL ** -0.5),
        "hgrn_lb_logits": nrm(ks[3], (DEPTH + 1, A_HEADS, A_DK), 0.1),
        "hgrn_norm_g": gain(ks[4], (DEPTH, A_DV)),
        "gdn_conv_w": nrm(ks[5], (DEPTH, CONV, B_CONV_CH), CONV ** -0.5),
        "gdn_A_log": jnp.log(jax.random.uniform(ks[6], (DEPTH, B_V_HEADS), f32,
                                                minval=1.0, maxval=16.0)),
        "gdn_dt_bias": dt + jnp.log(-jnp.expm1(-dt)),
        "gdn_norm_g": gain(ks[7], (DEPTH, B_DV)),
        "w_out": nrm(ks[9], (DEPTH, MIX_WIDTH, D_MODEL), MIX_WIDTH ** -0.5),
        "ffn_norm_g": gain(ks[10], (DEPTH, D_MODEL)),
        "peer_w_query": nrm(ks[11], (DEPTH, D_MODEL, P_HEADS, P_DKEY), D_MODEL ** -0.5),
        "peer_sub_keys": nrm(ks[12], (DEPTH, 2, P_HEADS, N_KEYS, P_DKEY // 2),
                             (P_DKEY // 2) ** -0.5),
        "peer_u": nrm(ks[13], (DEPTH, N_EXPERTS, D_MODEL), D_MODEL ** -0.5),
        "peer_v": nrm(ks[14], (DEPTH, N_EXPERTS, D_MODEL), P_HEADS ** -0.5),
        "final_norm_g": gain(ks[15], (D_MODEL,)),
    }


def reference(x, attn_norm_g, w_in, hgrn_lb_logits, hgrn_norm_g, gdn_conv_w, gdn_A_log,
              gdn_dt_bias, gdn_norm_g, w_out, ffn_norm_g, peer_w_query, peer_sub_keys,
              peer_u, peer_v, final_norm_g):
    b, s, _ = x.shape
    lb_all = jnp.cumsum(jax.nn.softmax(hgrn_lb_logits.astype(jnp.float32), axis=0), axis=0)
    offs = []
    acc = 0
    for n in IN_SIZES[:-1]:
        acc += n
        offs.append(acc)
    rep = B_V_HEADS // B_QK_HEADS
    h = x
    for l in range(DEPTH):
        xn = _rmsnorm(h, attn_norm_g[l])
        proj = xn @ w_in[l]
        a_q, a_f, a_i, a_g, b_q, b_k, b_v, b_z, b_b, b_a = jnp.split(proj, offs, axis=-1)
        o_a = _hgrn2(a_q.reshape(b, s, A_HEADS, A_DK), a_f.reshape(b, s, A_HEADS, A_DK),
                     a_i.reshape(b, s, A_HEADS, A_DV), lb_all[l])
        o_a = _gated_rmsnorm(o_a, a_g.reshape(b, s, A_HEADS, A_DV), hgrn_norm_g[l])
        qkv = _causal_conv_silu(jnp.concatenate([b_q, b_k, b_v], axis=-1), gdn_conv_w[l])
        nqk = B_QK_HEADS * B_DK
        gq = _l2norm(qkv[..., :nqk].reshape(b, s, B_QK_HEADS, B_DK))
        gk = _l2norm(qkv[..., nqk:2 * nqk].reshape(b, s, B_QK_HEADS, B_DK))
        gv = qkv[..., 2 * nqk:].reshape(b, s, B_V_HEADS, B_DV)
        gq = jnp.repeat(gq, rep, axis=2)
        gk = jnp.repeat(gk, rep, axis=2)
        beta = jax.nn.sigmoid(b_b.astype(jnp.float32))
        g = -jnp.exp(gdn_A_log[l].astype(jnp.float32)) * jax.nn.softplus(
            b_a.astype(jnp.float32) + gdn_dt_bias[l].astype(jnp.float32))
        o_b = _gated_delta(gq, gk, gv, beta, g)
        o_b = _gated_rmsnorm(o_b, b_z.reshape(b, s, B_V_HEADS, B_DV), gdn_norm_g[l])
        mix = jnp.concatenate([o_a.reshape(b, s, A_WIDTH), o_b.reshape(b, s, B_WIDTH)],
                              axis=-1).astype(x.dtype)
        h = h + mix @ w_out[l]
        hn = _rmsnorm(h, ffn_norm_g[l])
        h = h + _peer(hn, peer_w_query[l], peer_sub_keys[l], peer_u[l], peer_v[l])
    return _rmsnorm(h, final_norm_g)
```

```python
import numpy as np
from contextlib import ExitStack
import concourse.bass as bass
import concourse.mybir as mybir
from concourse.bass_utils import run_bass_kernel_spmd

F32 = mybir.dt.float32
BF16 = mybir.dt.bfloat16
I32 = mybir.dt.int32
AF = mybir.ActivationFunctionType
ALU = mybir.AluOpType

EPS = 1e-6
SEQ = 8192
D = 2048
NTA = 64
NST = 16
TOKB = 2048
NEG = -1.0e30

DEBUG = {}
PSUM_EXCL = True
NOSELF = False


class TK:
    def __init__(self, nc, es):
        self.nc = nc
        self.es = es
        self.eng = {'pe': nc.tensor, 'act': nc.scalar, 'dve': nc.vector, 'pool': nc.gpsimd, 'sp': nc.sync}
        self.sem = {k: es.enter_context(nc.semaphore("s_" + k)) for k in self.eng}
        self.cnt = {k: 0 for k in self.eng}
        self.waited = {k: {} for k in self.eng}
        self.dsem = {}
        self.dcnt = {}
        self.rings = {}
        self.lastw = {}
        self.readers = {}
        self.ninst = 0
        self.dead = False
        self.psum_names = set()

    @staticmethod
    def key(x):
        if isinstance(x, str):
            return x
        if hasattr(x, 'tensor'):
            return x.tensor.name
        return x.name

    @classmethod
    def keys(cls, x):
        k = cls.key(x)
        if k.startswith("tmpg") and hasattr(x, 'shape'):
            if x.shape[-1] == 128:
                return [f"{k}:{(x.offset % 512) // 128}"]
            return [f"{k}:{i}" for i in range(4)]
        return [k]

    def _wait(self, e, deps):
        if self.dead:
            return
        for k, v in deps.items():
            if k == e and (e == 'pe' or NOSELF):
                continue
            if self.waited[e].get(k, 0) >= v:
                continue
            sem = self.sem[k] if k in self.sem else self.dsem[k]
            self.eng[e].wait_ge(sem, v)
            self.waited[e][k] = v
            self.ninst += 1

    def _collect(self, reads, writes):
        deps = {}

        def add(tok):
            if tok is not None:
                deps[tok[0]] = max(deps.get(tok[0], 0), tok[1])
        for r in reads:
            for kr in self.keys(r):
                add(self.lastw.get(kr))
        for w in writes:
            for kw in self.keys(w):
                add(self.lastw.get(kw))
                for k, v in self.readers.get(kw, {}).items():
                    add((k, v))
        return deps

    def _record(self, tok, reads, writes):
        for r in reads:
            for kr in self.keys(r):
                d = self.readers.setdefault(kr, {})
                d[tok[0]] = max(d.get(tok[0], 0), tok[1])
        for w in writes:
            for kw in self.keys(w):
                self.lastw[kw] = tok
                self.readers[kw] = {}

    def op(self, e, fns, reads=(), writes=()):
        if self.dead:
            return None
        if PSUM_EXCL:
            ex = [r for r in reads if self.key(r) in self.psum_names]
            if ex:
                writes = list(writes) + ex
        if not isinstance(fns, (list, tuple)):
            fns = [fns]
        deps = self._collect(reads, writes)
        self._wait(e, deps)
        inst = None
        for f in fns:
            inst = f(self.eng[e])
            self.ninst += 1
        self.cnt[e] += 1
        inst.then_inc(self.sem[e], 1)
        self._record((e, self.cnt[e]), reads, writes)
        return inst

    def dma(self, q, chan, out, in_, extra_reads=(), extra_writes=(), **kw):
        if self.dead:
            return None
        ring = self.rings.setdefault(q, {'n': 40 if q == 'sp' else 8, 'i': 0})
        name = f"{q}{ring['i'] % ring['n']}"
        ring['i'] += 1
        if name not in self.dsem:
            self.dsem[name] = self.es.enter_context(self.nc.semaphore("d_" + name))
            self.dcnt[name] = 0
        reads = [in_] + list(extra_reads)
        writes = [out] + list(extra_writes)
        deps = self._collect(reads, writes)
        if self.dcnt[name] > 0:
            deps[name] = max(deps.get(name, 0), self.dcnt[name])
        self._wait(q, deps)
        inst = self.eng[q].dma_start(out=out, in_=in_, **kw)
        self.ninst += 1
        self.dcnt[name] += 16
        inst.then_inc(self.dsem[name], 16)
        self._record((name, self.dcnt[name]), reads, writes)
        return inst

    def barrier(self):
        allc = {k: v for k, v in self.cnt.items() if v > 0}
        allc.update({k: v for k, v in self.dcnt.items() if v > 0})
        for e in self.eng:
            self._wait(e, {k: v for k, v in allc.items() if k != e})


def build_program(debug=False, nst=NST, stop=''):
    nc = bass.Bass("TRN2", target_bir_lowering=False)

    def din(name, shape, dt=F32):
        if stop and stop[0] != 'B' and name in ("xs", "w_out", "w_query", "sk_t", "peer_u", "peer_v", "final_g"):
            return None
        if stop in ('B0', 'B1', 'B2') and name in ("peer_u", "peer_v", "final_g"):
            return None
        return nc.dram_tensor(name, list(shape), dt, kind="ExternalInput")

    x_d = din("x", [SEQ, D])
    xs_d = din("xs", [TOKB, D])
    win_d = din("w_in", [D, 1796])
    ag_d = din("attn_g", [128, 16])
    lbl_d = din("lb_logits", [128, 4])
    gain_d = din("gain512", [128, 512])
    cw_d = din("conv_w", [128, 16])
    al_d = din("a_log", [128, 2])
    dtb_d = din("dt_bias", [128, 2])
    wo_d = din("w_out", [D, D])
    fg_d = din("ffn_g", [128, 16])
    wq_d = din("w_query", [D, D])
    sk_d = din("sk_t", [128, 16 * 128])
    pu_d = din("peer_u", [16384, D])
    pv_d = din("peer_v", [16384, D])
    fin_d = din("final_g", [128, D])
    cst_d = din("consts", [128, 4 * 128])
    off_d = din("tok_off", [1, 1], I32)
    out_d = nc.dram_tensor("out", [TOKB, D], F32, kind="ExternalOutput")

    cin_ds = [nc.dram_tensor(f"cin{k}", [1024, 512], BF16) for k in range(8)]
    cout_d = nc.dram_tensor("cout", [8 * 4096 + 8192, 512], BF16)
    hscr_d = nc.dram_tensor("hscr", [TOKB, D], F32)
    hnscr_d = nc.dram_tensor("hnscr", [D, TOKB], BF16)
    gs2_d = nc.dram_tensor("gs2", [16, 128, 1024], F32)
    gc_d = nc.dram_tensor("gcc", [16, 128, 1024], F32)
    ga_d = nc.dram_tensor("gaa", [16, 128, 1024], F32)
    gp_d = nc.dram_tensor("gpp", [16, 128, 1024], BF16)
    utscr_d = nc.dram_tensor("utscr", [32, 128, 8192], BF16)
    vscr_d = nc.dram_tensor("vscr", [32, 128, 8192], BF16)

    dbg = {}

    def dout(name, shape, dt=F32):
        t = nc.dram_tensor(name, list(shape), dt, kind="ExternalOutput")
        dbg[name] = t
        return t

    with ExitStack() as top:
        tk = TK(nc, top)
        keep = []

        def chk(tag):
            if stop == tag and not tk.dead:
                tk.barrier()
                tk.dead = True

        def sb(es, name, shape, dt=F32):
            t = es.enter_context(nc.sbuf_tensor(name, list(shape), dt))
            keep.append(t)
            return t

        def ps(es, name, shape, dt=F32):
            t = es.enter_context(nc.psum_tensor(name, list(shape), dt))
            tk.psum_names.add(name)
            keep.append(t)
            return t

        def aps(*xs):
            return [a for a in xs if hasattr(a, 'tensor')]

        def ACT(out, in_, func, bias=0.0, scale=1.0, accum=None, eng='act'):
            kw = {}
            if accum is not None:
                kw['accum_out'] = accum
            tk.op('act', lambda e: e.activation(out=out, in_=in_, func=func, bias=bias, scale=scale, **kw),
                  reads=aps(in_, bias, scale), writes=aps(out, accum))

        def TS(out, in0, s1, op0, s2=None, op1=None, eng='dve'):
            if op1 is None:
                tk.op(eng, lambda e: e.tensor_scalar(out=out, in0=in0, scalar1=s1, scalar2=None, op0=op0),
                      reads=aps(in0, s1), writes=aps(out))
            else:
                tk.op(eng, lambda e: e.tensor_scalar(out=out, in0=in0, scalar1=s1, scalar2=s2, op0=op0, op1=op1),
                      reads=aps(in0, s1, s2), writes=aps(out))

        def TT(out, in0, in1, op, eng='dve'):
            tk.op(eng, lambda e: e.tensor_tensor(out=out, in0=in0, in1=in1, op=op), reads=aps(in0, in1), writes=aps(out))

        def STT(out, in0, scalar, in1, op0, op1):
            tk.op('dve', lambda e: e.scalar_tensor_tensor(out=out, in0=in0, scalar=scalar, in1=in1, op0=op0, op1=op1),
                  reads=aps(in0, scalar, in1), writes=aps(out))

        def CP(out, in_, eng='dve'):
            if eng == 'act':
                tk.op('act', lambda e: e.activation(out=out, in_=in_, func=AF.Copy), reads=aps(in_), writes=aps(out))
            else:
                tk.op(eng, lambda e: e.tensor_copy(out=out, in_=in_), reads=aps(in_), writes=aps(out))

        def RECIP(out, in_):
            tk.op('dve', lambda e: e.reciprocal(out=out, in_=in_), reads=aps(in_), writes=aps(out))

        def MM(out, pairs, extra_reads=(), first=True, last=True):
            n = len(pairs)
            fns = []
            rd = list(extra_reads)
            for i, (l, r) in enumerate(pairs):
                fns.append(lambda e, l=l, r=r, i=i: e.matmul(out, lhsT=l, rhs=r, start=(first and i == 0),
                                                             stop=(last and i == n - 1)))
                rd += [l, r]
            tk.op('pe', fns, reads=rd, writes=[out])

        def TRS(items):
            fns = []
            rd = []
            wr = []
            for (o, i, idn) in items:
                fns.append(lambda e, o=o, i=i, idn=idn: e.transpose(out=o, in_=i, identity=idn))
                rd += [i, idn]
                wr.append(o)
            tk.op('pe', fns, reads=rd, writes=wr)

        def DMA(out, in_, q='sp', chan='ld', **kw):
            tk.dma(q, chan, out, in_, **kw)

        def rstd_from_ss(rs, ss, n, tmp):
            TS(tmp, ss, 1.0 / n, ALU.mult, EPS, ALU.add)
            ACT(tmp, tmp, AF.Sqrt)
            RECIP(rs, tmp)

        cst = sb(top, "cst", [128, 4, 128])
        cstb = sb(top, "cstb", [128, 4, 128], BF16)
        DMA(cst[:].rearrange("p a b -> p (a b)"), cst_d[:, :])
        CP(cstb[:], cst[:])
        ident, maskU, strictU, ones = (cst[:, i, :] for i in range(4))
        identb, maskUb, strictUb, onesb = (cstb[:, i, :] for i in range(4))

        with ExitStack() as pa:
            wsb = sb(pa, "wsb", [128, 16, 1796], BF16)
            agsb = sb(pa, "agsb", [128, 16])
            DMA(agsb[:], ag_d[:, :])
            with ExitStack() as tmpes:
                wst = [sb(tmpes, f"wst{i}", [128, 1796]) for i in range(2)]
                for dc in range(16):
                    DMA(wst[dc % 2][:], win_d[dc * 128:(dc + 1) * 128, :])
                    TS(wsb[:, dc, :], wst[dc % 2][:], agsb[:, dc:dc + 1], ALU.mult, eng=('dve' if dc % 2 == 0 else 'pool'))
                tk.barrier()
            lbl = sb(pa, "lbl", [128, 4])
            oml = sb(pa, "oml", [128, 2])
            gain = sb(pa, "gain", [128, 512])
            cw = sb(pa, "cw", [128, 4, 4])
            nexpA = sb(pa, "nexpA", [128, 2])
            dtb = sb(pa, "dtb", [128, 2])
            DMA(lbl[:], lbl_d[:, :])
            DMA(gain[:], gain_d[:, :])
            DMA(cw[:].rearrange("p a b -> p (a b)"), cw_d[:, :])
            DMA(nexpA[:], al_d[:, :])
            DMA(dtb[:], dtb_d[:, :])
            TT(oml[:], lbl[:, 2:4], lbl[:, 0:2], ALU.subtract)
            ACT(oml[:], oml[:], AF.Sigmoid)
            ACT(nexpA[:], nexpA[:], AF.Exp)
            TS(nexpA[:], nexpA[:], -1.0, ALU.mult)
            onesf = sb(pa, "onesf", [128, 128])
            tk.op('pool', lambda e: e.memset(onesf[:], 1.0), writes=[onesf])

            xt = [sb(pa, f"xt{i}", [128, D]) for i in range(2)]
            xb = sb(pa, "xb", [128, D], BF16)
            ss1 = sb(pa, "ss1", [128, 1])
            rs1 = sb(pa, "rs1", [128, 1])
            tm1 = sb(pa, "tm1", [128, 1])
            xnT = sb(pa, "xnT", [128, 16, 512], BF16)
            qT = [sb(pa, f"qT{h}", [128, 512]) for h in range(2)]
            kT = [sb(pa, f"kT{h}", [128, 512]) for h in range(2)]
            lgf = [sb(pa, f"lgf{h}", [128, 512]) for h in range(2)]
            bT = [sb(pa, f"bT{h}", [128, 512]) for h in range(2)]
            etmp = sb(pa, "etmp", [128, 512])
            qeT = [sb(pa, f"qeT{h}", [128, 512], BF16) for h in range(2)]
            kbT = [sb(pa, f"kbT{h}", [128, 512], BF16) for h in range(2)]
            kdT = [sb(pa, f"kdT{h}", [128, 512], BF16) for h in range(2)]
            dec = [sb(pa, f"dec{h}", [128, 4]) for h in range(2)]
            cb = [sb(pa, f"cb{i}", [128, 515]) for i in range(4)]
            cacc = sb(pa, "cacc", [128, 512])
            csl = [sb(pa, f"csl{i}", [128, 512]) for i in range(2)]
            sqb = sb(pa, "sqb", [128, 512], BF16)
            rn = sb(pa, "rn", [128, 512])
            qnT = sb(pa, "qnT", [128, 512], BF16)
            knT = sb(pa, "knT", [128, 512], BF16)
            vT = [sb(pa, f"vT{i}", [128, 512], BF16) for i in range(2)]
            vA = [sb(pa, f"vA{j}", [128, 256], BF16) for j in range(4)]
            gzs = [sb(pa, f"gzs{j}", [128, 512]) for j in range(4)]
            bba = [sb(pa, f"bba{j}", [128, 4]) for j in range(4)]
            SA = [sb(pa, f"SA{h}", [128, 128]) for h in range(2)]
            SAb = [sb(pa, f"SAb{h}", [128, 128], BF16) for h in range(2)]
            SB = [sb(pa, f"SB{h}", [128, 128]) for h in range(2)]
            SBb = [sb(pa, f"SBb{h}", [128, 128], BF16) for h in range(2)]
            for t in SA + SB + SAb + SBb:
                tk.op('pool', lambda e, t=t: e.memset(t[:], 0.0), writes=[t])
            for i in range(4):
                tk.op('pool', lambda e, i=i: e.memset(cb[i][:, 0:3], 0.0), writes=[cb[i]])
            attb2 = [[sb(pa, f"attb{h}{p}", [128, 128], BF16) for p in range(2)] for h in range(2)]
            kdb2 = [[sb(pa, f"kdb{h}{p}", [128, 128], BF16) for p in range(2)] for h in range(2)]
            oall = sb(pa, "oall", [128, 512])
            ss4 = sb(pa, "ss4", [128, 4])
            rs4 = sb(pa, "rs4", [128, 4])
            tm4 = sb(pa, "tm4", [128, 4])
            junk = sb(pa, "junk", [128, 128])
            mixt = [sb(pa, f"mixt{i}", [128, 512], BF16) for i in range(2)]
            KKm = sb(pa, "KKm", [128, 128])
            QKm = sb(pa, "QKm", [128, 128])
            g2 = sb(pa, "g2", [128, 2])
            beta2 = sb(pa, "beta2", [128, 2])
            sp2 = sb(pa, "sp2", [128, 2])
            gcs = sb(pa, "gcs", [128, 2])
            gls = sb(pa, "gls", [128, 2])
            egc2 = [sb(pa, f"egc{p}", [128, 2]) for p in range(2)]
            ekd = sb(pa, "ekd", [128, 2])
            dl2 = [sb(pa, f"dl{p}", [128, 2]) for p in range(2)]
            bge = sb(pa, "bge", [128, 2])
            dg = [sb(pa, f"dg{v}", [128, 256]) for v in range(2)]
            Dm = [sb(pa, f"Dm{v}", [128, 128]) for v in range(2)]
            Ee = [sb(pa, f"Ee{v}", [128, 128]) for v in range(2)]
            aqk2 = [[sb(pa, f"aqk{v}{p}", [128, 128], BF16) for p in range(2)] for v in range(2)]
            Xk = [sb(pa, f"Xk{v}", [128, 128]) for v in range(2)]
            Pa = [[sb(pa, f"Pa{v}{i}", [128, 128]) for i in range(2)] for v in range(2)]
            Pt = [[sb(pa, f"Pt{v}{i}", [128, 128]) for i in range(2)] for v in range(2)]
            Tt = [[sb(pa, f"Tt{v}{i}", [128, 128]) for i in range(2)] for v in range(2)]
            Ttb = [sb(pa, f"Ttb{v}", [128, 128], BF16) for v in range(2)]
            vbt = [sb(pa, f"vbt{v}", [128, 128], BF16) for v in range(2)]
            kbg = [sb(pa, f"kbg{v}", [128, 128], BF16) for v in range(2)]
            kdg2 = [[sb(pa, f"kdg{v}{p}", [128, 128], BF16) for p in range(2)] for v in range(2)]
            us2 = [[sb(pa, f"us{v}{p}", [128, 128]) for p in range(2)] for v in range(2)]
            wTb2 = [[sb(pa, f"wTb{v}{p}", [128, 128], BF16) for p in range(2)] for v in range(2)]
            vnew = [sb(pa, f"vnew{v}", [128, 128], BF16) for v in range(2)]
            o1 = [sb(pa, f"o1{v}", [128, 128]) for v in range(2)]

            ptr = [ps(pa, f"ptr{i}", [128, 1024], BF16) for i in range(2)]
            pbig = [ps(pa, f"pbig{i}", [128, 512]) for i in range(3)]
            psmb = [ps(pa, f"psm{i}", [128, 4, 128]) for i in range(3)]
            psm_i = [0]

            class Sub:
                def __init__(self, ap):
                    self.ap = ap

                def __getitem__(self, idx):
                    assert idx == slice(None)
                    return self.ap

            def small():
                k = psm_i[0] % 12
                psm_i[0] += 1
                return Sub(psmb[k % 3][:, k // 3, :])
            pbig_i = [0]

            def big():
                t = pbig[pbig_i[0] % 3]
                pbig_i[0] += 1
                return t

            for st in range(nst):
                for j in range(4):
                    tt = st * 4 + j
                    xtile = xt[tt % 2]
                    DMA(xtile[:], x_d[tt * 128:(tt + 1) * 128, :])
                    ACT(xb[:], xtile[:], AF.Square, accum=ss1[:])
                    rstd_from_ss(rs1[:], ss1[:], float(D), tm1[:])
                    ACT(xb[:], xtile[:], AF.Copy, scale=rs1[:])
                    for hf in range(2):
                        TRS([(ptr[hf][:, i * 128:(i + 1) * 128], xb[:, (hf * 8 + i) * 128:(hf * 8 + i + 1) * 128], identb)
                             for i in range(8)])
                        CP(xnT[:, hf * 8:(hf + 1) * 8, j * 128:(j + 1) * 128],
                           ptr[hf][:].rearrange("p (a b) -> p a b", b=128), eng=('dve' if hf == 0 else 'act'))
                chk('A1')
                for blk in range(8):
                    pb = big()
                    MM(pb[:], [(wsb[:, dc, blk * 128:(blk + 1) * 128], xnT[:, dc, :]) for dc in range(16)])
                    if blk < 2:
                        CP(qT[blk][:], pb[:], eng='act')
                    elif blk < 4:
                        h = blk - 2
                        ACT(kT[h][:], pb[:], AF.Sigmoid, scale=-1.0)
                        TS(kT[h][:], kT[h][:], oml[:, h:h + 1], ALU.mult)
                        ACT(lgf[h][:], kT[h][:], AF.Ln, scale=-1.0, bias=1.0)
                    else:
                        CP(cb[blk - 4][:, 3:515], pb[:], eng='act')
                chk('A2')
                for j in range(4):
                    pb = big()
                    MM(pb[:], [(xnT[:, dc, j * 128:(j + 1) * 128], wsb[:, dc, 1024:1536]) for dc in range(16)])
                    CP(vA[j][:], pb[:, 0:256])
                    ACT(gzs[j][:, 0:256], pb[:, 256:512], AF.Silu)
                    pb2 = big()
                    MM(pb2[:, 0:260], [(xnT[:, dc, j * 128:(j + 1) * 128], wsb[:, dc, 1536:1796]) for dc in range(16)])
                    ACT(gzs[j][:, 256:512], pb2[:, 0:256], AF.Silu)
                    CP(bba[j][:], pb2[:, 256:260])
                    TT(gzs[j][:], gzs[j][:], gain[:], ALU.mult)
                chk('A3')
                for h in range(2):
                    for c in range(4):
                        sl = slice(c * 128, (c + 1) * 128)
                        tk.op('dve', lambda e, h=h, sl=sl: e.tensor_tensor_scan(
                            out=bT[h][:, sl], data0=onesf[:], data1=lgf[h][:, sl], initial=0.0,
                            op0=ALU.mult, op1=ALU.add), reads=[onesf, lgf[h]], writes=[bT[h]])
                    ACT(etmp[:], bT[h][:], AF.Exp)
                    TT(qeT[h][:], qT[h][:], etmp[:], ALU.mult)
                    ACT(etmp[:], bT[h][:], AF.Exp, scale=-1.0)
                    TT(kbT[h][:], kT[h][:], etmp[:], ALU.mult)
                    for c in range(4):
                        sl = slice(c * 128, (c + 1) * 128)
                        ACT(etmp[:, sl], bT[h][:, sl], AF.Exp, scale=-1.0, bias=bT[h][:, c * 128 + 127:c * 128 + 128])
                        ACT(dec[h][:, c:c + 1], bT[h][:, c * 128 + 127:c * 128 + 128], AF.Exp)
                    TT(kdT[h][:], kT[h][:], etmp[:], ALU.mult)
                chk('A4')
                for i in range(4):
                    TS(cacc[:], cb[i][:, 3:515], cw[:, i, 3:4], ALU.mult)
                    for tap in range(3):
                        STT(cacc[:], cb[i][:, tap:tap + 512], cw[:, i, tap:tap + 1], cacc[:], ALU.mult, ALU.add)
                    CP(cb[i][:, 0:3], cb[i][:, 512:515], eng='pool')
                    if i < 2:
                        ACT(csl[i][:], cacc[:], AF.Silu)
                        ACT(sqb[:], csl[i][:], AF.Square)
                        pb = big()
                        MM(pb[:], [(onesb, sqb[:])])
                        ACT(rn[:], pb[:], AF.Sqrt, bias=EPS)
                        RECIP(rn[:], rn[:])
                        if i == 0:
                            STT(qnT[:], csl[0][:], 128.0 ** -0.5, rn[:], ALU.mult, ALU.mult)
                        else:
                            TT(knT[:], csl[1][:], rn[:], ALU.mult)
                    else:
                        ACT(vT[i - 2][:], cacc[:], AF.Silu)

                chk('A5')
                def prepH(c):
                    sl = slice(c * 128, (c + 1) * 128)
                    p = c % 2
                    for h in range(2):
                        pa_ = small()
                        MM(pa_[:], [(kbT[h][:, sl], qeT[h][:, sl])])
                        TT(attb2[h][p][:], pa_[:], maskU, ALU.mult)
                        pk = ptr[0]
                        TRS([(pk[:, 0:128], kdT[h][:, sl], identb)])
                        CP(kdb2[h][p][:], pk[:, 0:128], eng='act')

                def seqH(c):
                    sl = slice(c * 128, (c + 1) * 128)
                    p = c % 2
                    for h in range(2):
                        po = small()
                        MM(po[:], [(qeT[h][:, sl], SAb[h][:]), (attb2[h][p][:], vA[c][:, h * 128:(h + 1) * 128])])
                        CP(oall[:, h * 128:(h + 1) * 128], po[:], eng='act')
                        p4 = small()
                        MM(p4[:], [(kdb2[h][p][:], vA[c][:, h * 128:(h + 1) * 128])])
                        STT(SA[h][:], SA[h][:], dec[h][:, c:c + 1], p4[:], ALU.mult, ALU.add)
                        CP(SAb[h][:], SA[h][:], eng='act')

                def prepG(c):
                    sl = slice(c * 128, (c + 1) * 128)
                    p = c % 2
                    egc, dl = egc2[p], dl2[p]
                    ACT(beta2[:], bba[c][:, 0:2], AF.Sigmoid)
                    TT(sp2[:], bba[c][:, 2:4], dtb[:], ALU.add)
                    ACT(sp2[:], sp2[:], AF.Exp)
                    ACT(sp2[:], sp2[:], AF.Ln, bias=1.0)
                    TT(g2[:], sp2[:], nexpA[:], ALU.mult)
                    pg = big()
                    MM(pg[:, 0:2], [(maskU, g2[:])])
                    MM(pg[:, 2:4], [(ones, g2[:])])
                    CP(gcs[:], pg[:, 0:2])
                    CP(gls[:], pg[:, 2:4])
                    ACT(egc[:], gcs[:], AF.Exp)
                    ACT(dl[:], gls[:], AF.Exp)
                    TT(ekd[:], gls[:], gcs[:], ALU.subtract)
                    ACT(ekd[:], ekd[:], AF.Exp)
                    TT(bge[:], beta2[:], egc[:], ALU.mult)
                    pkk = small()
                    MM(pkk[:], [(knT[:, sl], knT[:, sl])])
                    TT(KKm[:], pkk[:], strictU, ALU.mult)
                    pqk = small()
                    MM(pqk[:], [(knT[:, sl], qnT[:, sl])])
                    TT(QKm[:], pqk[:], maskU, ALU.mult)
                    pkt = ptr[1]
                    TRS([(pkt[:, 0:128], knT[:, sl], identb),
                         (pkt[:, 128:256], vT[0][:, sl], identb),
                         (pkt[:, 256:384], vT[1][:, sl], identb)])
                    for v in range(2):
                        TS(vbt[v][:], pkt[:, 128 * (v + 1):128 * (v + 2)], beta2[:, v:v + 1], ALU.mult)
                        TS(kbg[v][:], pkt[:, 0:128], bge[:, v:v + 1], ALU.mult)
                        TS(kdg2[v][p][:], pkt[:, 0:128], ekd[:, v:v + 1], ALU.mult)
                    for v in range(2):
                        TS(dg[v][:, 0:128], ident, gcs[:, v:v + 1], ALU.mult, eng='pool')
                        TS(dg[v][:, 128:256], ident, beta2[:, v:v + 1], ALU.mult, eng='pool')
                        prow = big()
                        MM(prow[:, 0:256], [(ones, dg[v][:])])
                        TS(Dm[v][:], prow[:, 0:128], gcs[:, v:v + 1], ALU.subtract, 0.0, ALU.min)
                        ACT(Ee[v][:], Dm[v][:], AF.Exp)
                        TT(aqk2[v][p][:], QKm[:], Ee[v][:], ALU.mult)
                        TT(Xk[v][:], KKm[:], Ee[v][:], ALU.mult)
                        TT(Pa[v][0][:], Xk[v][:], prow[:, 128:256], ALU.mult)
                        pT0 = small()
                        TRS([(pT0[:], Pa[v][0][:], ident)])
                        CP(Pt[v][0][:], pT0[:], eng='act')
                        TT(Tt[v][0][:], ident, Pa[v][0][:], ALU.subtract)
                    cur = 0
                    for lev in range(6):
                        nxt = 1 - cur
                        for v in range(2):
                            last = (lev == 5)
                            pPt = small()
                            MM(pPt[:], [(Pa[v][cur][:], Pt[v][cur][:])])
                            CP(Pt[v][nxt][:], pPt[:], eng='act')
                            if not last:
                                pP = small()
                                MM(pP[:], [(Pt[v][cur][:], Pa[v][cur][:])])
                                CP(Pa[v][nxt][:], pP[:], eng='act')
                            pTT = small()
                            MM(pTT[:], [(Pt[v][nxt][:], Tt[v][cur][:])])
                            if not last:
                                TT(Tt[v][nxt][:], Tt[v][cur][:], pTT[:], ALU.add)
                            else:
                                TT(Ttb[v][:], Tt[v][cur][:], pTT[:], ALU.add)
                        cur = nxt
                    for v in range(2):
                        pu = small()
                        MM(pu[:], [(Ttb[v][:], vbt[v][:])])
                        CP(us2[v][p][:], pu[:], eng='act')
                        pw = small()
                        MM(pw[:], [(kbg[v][:], Ttb[v][:])])
                        CP(wTb2[v][p][:], pw[:], eng='act')

                def seqG(c):
                    sl = slice(c * 128, (c + 1) * 128)
                    p = c % 2
                    egc, dl = egc2[p], dl2[p]
                    for v in range(2):
                        p1 = small()
                        MM(p1[:], [(wTb2[v][p][:], SBb[v][:])])
                        TT(vnew[v][:], us2[v][p][:], p1[:], ALU.subtract)
                        p2 = small()
                        MM(p2[:], [(qnT[:, sl], SBb[v][:])])
                        p3 = small()
                        MM(p3[:], [(aqk2[v][p][:], vnew[v][:])])
                        TS(o1[v][:], p2[:], egc[:, v:v + 1], ALU.mult)
                        TT(oall[:, (2 + v) * 128:(3 + v) * 128], o1[v][:], p3[:], ALU.add)
                        p4 = small()
                        MM(p4[:], [(kdg2[v][p][:], vnew[v][:])])
                        STT(SB[v][:], SB[v][:], dl[:, v:v + 1], p4[:], ALU.mult, ALU.add)
                        CP(SBb[v][:], SB[v][:], eng='act')

                def fin(c):
                    tt = st * 4 + c
                    for hh in range(4):
                        ACT(junk[:], oall[:, hh * 128:(hh + 1) * 128], AF.Square, accum=ss4[:, hh:hh + 1])
                    rstd_from_ss(rs4[:], ss4[:], 128.0, tm4[:])
                    mt = mixt[tt % 2]
                    for hh in range(4):
                        STT(mt[:, hh * 128:(hh + 1) * 128], oall[:, hh * 128:(hh + 1) * 128], rs4[:, hh:hh + 1],
                            gzs[c][:, hh * 128:(hh + 1) * 128], ALU.mult, ALU.mult)
                    DMA(cin_ds[tt // 8][(tt % 8) * 128:(tt % 8 + 1) * 128, :], mt[:], chan='cin')
                    if debug and tt < 2:
                        od = dout(f"dbg_oall{tt}", [128, 512])
                        DMA(od[:, :], oall[:], chan='dbg')

                prepH(0)
                prepG(0)
                for c in range(4):
                    if c + 1 < 4:
                        prepH(c + 1)
                    seqH(c)
                    if c + 1 < 4:
                        prepG(c + 1)
                    seqG(c)
                    fin(c)
            tk.barrier()
        if stop and stop[0] != 'B':
            tk.dead = False
            od = dout("dbg_mix", [nst * 512, 512], BF16)
            for k in range((nst + 1) // 2):
                nr = min(1024, nst * 512 - k * 1024)
                tk.dma('sp', 'dbg', od[k * 1024:k * 1024 + nr, :], cin_ds[k][0:nr, :])
            tk._wait('sp', {k: v for k, v in tk.dcnt.items()})
            return nc, dbg

        if not tk.dead:
            for k in range(8):
                ccs = top.enter_context(nc.semaphore(f"ccs{k}"))
                nc.gpsimd.collective_compute("AllGather", ALU.bypass, replica_groups=[[0, 1, 2, 3], [4, 5, 6, 7]],
                                             ins=[cin_ds[k].ap().opt()],
                                             outs=[cout_d.ap()[k * 4096:(k + 1) * 4096, :].opt()]).then_inc(ccs)
                nc.gpsimd.wait_ge(ccs, 1)
        offt = sb(top, "offt", [1, 1], I32)
        DMA(offt[:], off_d[:, :], q='pool', chan='pl')
        tk._wait('pool', {k: v for k, v in tk.dcnt.items()})
        reg = top.enter_context(nc.gpsimd.register("roff"))
        if not tk.dead:
            nc.gpsimd.reg_load(reg, offt[0:1, 0:1])
        ov = nc.gpsimd.snap(reg)
        def cview(j):
            s0 = (j // 8) * 4096 + (j % 8) * 128
            R = cout_d.ap()[s0:s0 + 28672, :]
            return R[bass.ds(ov, 4096), :].rearrange("(r t) n -> t r n", r=4)[0:128, :, :]
        if stop == 'B0':
            od = dout("dbg_cout", [4 * 512, 512], BF16)
            tk.dma('sp', 'dbg', od[:, :], cout_d[0:2048, :])
            tk._wait('sp', {k: v for k, v in tk.dcnt.items()})
            return nc, dbg

        with ExitStack() as pb_:
            wo = sb(pb_, "wo", [128, 16, D], BF16)
            fgs = sb(pb_, "fgs", [128, 16])
            DMA(fgs[:], fg_d[:, :])
            with ExitStack() as tmpes:
                wst = [sb(tmpes, f"wost{i}", [128, D]) for i in range(2)]
                for cc in range(16):
                    DMA(wst[cc % 2][:], wo_d[cc * 128:(cc + 1) * 128, :])
                    CP(wo[:, cc, :], wst[cc % 2][:], eng=('act' if cc % 2 == 0 else 'pool'))
                tk.barrier()
            mixg = [sb(pb_, f"mixg{i}", [128, 4, 512], BF16) for i in range(2)]
            mixT = sb(pb_, "mixT", [128, 16, 128], BF16)
            xres = [sb(pb_, f"xres{i}", [128, D]) for i in range(2)]
            hsb = [sb(pb_, f"hsb{i}", [128, D]) for i in range(2)]
            hb = sb(pb_, "hb", [128, D], BF16)
            ss1 = sb(pb_, "ss1b", [128, 1])
            rs1 = sb(pb_, "rs1b", [128, 1])
            tm1 = sb(pb_, "tm1b", [128, 1])
            hnTj = [sb(pb_, f"hnTj{i}", [128, 16, 128], BF16) for i in range(2)]
            ptr = [ps(pb_, f"ptrb{i}", [128, 1024], BF16) for i in range(2)]
            po = [ps(pb_, f"pob{i}", [128, 512]) for i in range(4)]
            for j in range(16):
                mg = mixg[j % 2]
                tk.dma('pool', 'pl', mg[:], cview(j), extra_reads=[cout_d])
                DMA(xres[j % 2][:], xs_d[j * 128:(j + 1) * 128, :])
                for hf in range(2):
                    TRS([(ptr[hf][:, i * 128:(i + 1) * 128], mg[:, hf * 2 + i // 4, (i % 4) * 128:(i % 4 + 1) * 128], identb)
                         for i in range(8)])
                    CP(mixT[:, hf * 8:(hf + 1) * 8, :], ptr[hf][:].rearrange("p (a b) -> p a b", b=128),
                       eng=('dve' if hf == 0 else 'act'))
                hs_ = hsb[j % 2]
                for n in range(4):
                    MM(po[n][:], [(mixT[:, cc, :], wo[:, cc, n * 512:(n + 1) * 512]) for cc in range(16)])
                    TT(hs_[:, n * 512:(n + 1) * 512], po[n][:], xres[j % 2][:, n * 512:(n + 1) * 512], ALU.add)
                DMA(hscr_d[j * 128:(j + 1) * 128, :], hs_[:], chan='st')
                ACT(hb[:], hs_[:], AF.Square, accum=ss1[:])
                rstd_from_ss(rs1[:], ss1[:], float(D), tm1[:])
                ACT(hb[:], hs_[:], AF.Copy, scale=rs1[:])
                hT = hnTj[j % 2]
                for hf in range(2):
                    TRS([(ptr[hf][:, i * 128:(i + 1) * 128], hb[:, (hf * 8 + i) * 128:(hf * 8 + i + 1) * 128], identb)
                         for i in range(8)])
                    for i in range(8):
                        dc = hf * 8 + i
                        TS(hT[:, dc, :], ptr[hf][:, i * 128:(i + 1) * 128], fgs[:, dc:dc + 1], ALU.mult,
                           eng='dve')
                DMA(hnscr_d.ap().rearrange("(c p) t -> p c t", p=128)[:, :, j * 128:(j + 1) * 128], hT[:], chan='st')
            tk.barrier()

        if stop == 'B1':
            od = dout("dbg_h", [TOKB, D])
            tk.dma('sp', 'dbg', od[:, :], hscr_d[:, :])
            od2 = dout("dbg_hn", [D, TOKB], BF16)
            tk.dma('sp', 'dbg', od2[:, :], hnscr_d[:, :])
            tk._wait('sp', {k: v for k, v in tk.dcnt.items()})
            return nc, dbg
        with ExitStack() as pc_:
            wq = sb(pc_, "wq", [128, 16, D], BF16)
            skT = sb(pc_, "skT", [128, 16, 128], BF16)
            with ExitStack() as tmpes:
                wst = [sb(tmpes, f"wqst{i}", [128, D]) for i in range(2)]
                for cc in range(16):
                    DMA(wst[cc % 2][:], wq_d[cc * 128:(cc + 1) * 128, :])
                    CP(wq[:, cc, :], wst[cc % 2][:], eng=('act' if cc % 2 == 0 else 'pool'))
                DMA(wst[0][:], sk_d[:, :])
                CP(skT[:].rearrange("p a b -> p (a b)"), wst[0][:])
                tk.barrier()
            hT2 = [sb(pc_, f"hT2{i}", [128, 16, 128], BF16) for i in range(2)]
            qTs = sb(pc_, "qTs", [128, 16, 128], BF16)
            sc = [sb(pc_, f"sc{i}", [128, 16, 128]) for i in range(2)]
            scr = sb(pc_, "scr", [128, 128])
            v12 = sb(pc_, "v12", [128, 16, 16])
            cand = sb(pc_, "cand", [128, 16, 16])
            scr2 = sb(pc_, "scr2", [128, 256])
            c8 = sb(pc_, "c8", [128, 24])
            thr = sb(pc_, "thr", [128, 1])
            nm = sb(pc_, "nm", [128, 1])
            ej = sb(pc_, "ej", [128, 16])
            Zs = sb(pc_, "Zs", [128, 1])
            rZ = sb(pc_, "rZ", [128, 1])
            nm1 = sb(pc_, "nm1", [128, 1])
            nm2 = sb(pc_, "nm2", [128, 1])
            E1 = sb(pc_, "E1", [128, 128])
            ga = [sb(pc_, f"ga{i}", [128, 8, 128]) for i in range(2)]
            gp = [sb(pc_, f"gp{i}", [128, 8, 128], BF16) for i in range(2)]
            gc_ = [sb(pc_, f"gc{i}", [128, 8, 128]) for i in range(2)]
            pq = [ps(pc_, f"pq{i}", [128, 4, 128]) for i in range(4)]
            psc = [ps(pc_, f"psc{i}", [128, 4, 128]) for i in range(4)]
            for j in range(16):
                hT = hT2[j % 2]
                DMA(hT[:], hnscr_d.ap().rearrange("(c p) t -> p c t", p=128)[:, :, j * 128:(j + 1) * 128])
                for q4 in range(4):
                    for bi in range(4):
                        blk = q4 * 4 + bi
                        MM(pq[q4][:, bi, :], [(wq[:, dc, blk * 128:(blk + 1) * 128], hT[:, dc, :]) for dc in range(16)])
                    CP(qTs[:, q4 * 4:(q4 + 1) * 4, :], pq[q4][:], eng='act')
                scj = sc[j % 2]
                for q4 in range(4):
                    for bi in range(4):
                        blk = q4 * 4 + bi
                        MM(psc[q4][:, bi, :], [(qTs[:, blk, :], skT[:, blk, :])])
                    CP(scj[:, q4 * 4:(q4 + 1) * 4, :], psc[q4][:], eng='act')
                for blk in range(16):
                    tk.op('dve', lambda e, blk=blk: e.max(out=v12[:, blk, 0:8], in_=scj[:, blk, :]), reads=[scj], writes=[v12])
                    tk.op('dve', lambda e, blk=blk: e.match_replace(out=scr[:], in_to_replace=v12[:, blk, 0:8],
                                                                   in_values=scj[:, blk, :], imm_value=NEG),
                          reads=[scj, v12], writes=[scr])
                    tk.op('dve', lambda e, blk=blk: e.max(out=v12[:, blk, 8:16], in_=scr[:]), reads=[scr], writes=[v12])
                gaj, gpj, gcj = ga[j % 2], gp[j % 2], gc_[j % 2]
                for h in range(8):
                    for k in range(16):
                        TS(cand[:, k, :], v12[:, 8 + h, :], v12[:, h, k:k + 1], ALU.add)
                    cf = cand[:].rearrange("p a b -> p (a b)")
                    tk.op('dve', lambda e: e.max(out=c8[:, 0:8], in_=cf), reads=[cand], writes=[c8])
                    tk.op('dve', lambda e: e.match_replace(out=scr2[:], in_to_replace=c8[:, 0:8], in_values=cf, imm_value=NEG),
                          reads=[cand, c8], writes=[scr2])
                    tk.op('dve', lambda e: e.max(out=c8[:, 8:16], in_=scr2[:]), reads=[scr2], writes=[c8])
                    tk.op('dve', lambda e: e.match_replace(out=scr2[:], in_to_replace=c8[:, 8:16], in_values=scr2[:], imm_value=NEG),
                          reads=[scr2, c8], writes=[scr2])
                    tk.op('dve', lambda e: e.max(out=c8[:, 16:24], in_=scr2[:]), reads=[scr2], writes=[c8])
                    TT(thr[:], c8[:, 15:16], c8[:, 16:17], ALU.add)
                    TS(thr[:], thr[:], 0.5, ALU.mult)
                    TS(nm[:], c8[:, 0:1], -1.0, ALU.mult)
                    ACT(ej[:], c8[:, 0:16], AF.Exp, bias=nm[:], accum=Zs[:])
                    RECIP(rZ[:], Zs[:])
                    TS(nm1[:], v12[:, h, 0:1], -1.0, ALU.mult)
                    TS(nm2[:], v12[:, 8 + h, 0:1], -1.0, ALU.mult)
                    ACT(E1[:], scj[:, h, :], AF.Exp, bias=nm1[:])
                    STT(E1[:], scj[:, h, :], v12[:, h, 15:16], E1[:], ALU.is_ge, ALU.mult)
                    TS(gaj[:, h, :], E1[:], rZ[:], ALU.mult)
                    ACT(E1[:], scj[:, 8 + h, :], AF.Exp, bias=nm2[:])
                    STT(gpj[:, h, :], scj[:, 8 + h, :], v12[:, 8 + h, 15:16], E1[:], ALU.is_ge, ALU.mult)
                    TS(gcj[:, h, :], scj[:, h, :], -1.0, ALU.mult, thr[:], ALU.add)
                DMA(gs2_d[j], scj[:, 8:16, :].rearrange("p a b -> p (a b)"), chan='st')
                DMA(gc_d[j], gcj[:].rearrange("p a b -> p (a b)"), chan='st')
                DMA(ga_d[j], gaj[:].rearrange("p a b -> p (a b)"), chan='st')
                DMA(gp_d[j], gpj[:].rearrange("p a b -> p (a b)"), chan='st')
                if debug and j == 0:
                    od = dout("dbg_sc", [128, 2048])
                    DMA(od[:, :], scj[:].rearrange("p a b -> p (a b)"), chan='dbg')
            tk.barrier()

        if stop == 'B2':
            for nm_, t_, dt_ in (("dbg_gs2", gs2_d, F32), ("dbg_gc", gc_d, F32), ("dbg_ga", ga_d, F32), ("dbg_gp", gp_d, BF16)):
                od = dout(nm_, [16, 128, 1024], dt_)
                tk.dma('sp', 'dbg', od[:, :, :], t_[:, :, :])
            tk._wait('sp', {k: v for k, v in tk.dcnt.items()})
            return nc, dbg
        with ExitStack() as pd_:
            fing = sb(pd_, "fing", [128, D])
            DMA(fing[:], fin_d[:, :])
            acc = sb(pd_, "acc", [128, 4, D])
            hnT = sb(pd_, "hnT", [128, 16, 512], BF16)
            s2t = sb(pd_, "s2t", [128, 4, 1024])
            cct = sb(pd_, "cct", [128, 4, 1024])
            gat = sb(pd_, "gat", [128, 4, 1024])
            gpt = sb(pd_, "gpt", [128, 4, 1024], BF16)
            ust = [sb(pd_, f"ust{i}", [128, D]) for i in range(2)]
            UT = sb(pd_, "UT", [128, 16, 512], BF16)
            vst = sb(pd_, "vst", [128, D])
            Vb = [sb(pd_, f"Vb{i}", [128, 4, D], BF16) for i in range(2)]
            hg = [sb(pd_, f"hg{i}", [128, 512], BF16) for i in range(3)]
            tmpg = [sb(pd_, f"tmpg{i}", [128, 512], BF16) for i in range(8)]
            Am = [sb(pd_, f"Am{i}", [128, 512], BF16) for i in range(2)]
            AT = [sb(pd_, f"AT{i}", [128, 4, 128], BF16) for i in range(2)]
            ss1 = sb(pd_, "ss1d", [128, 1])
            rs1 = sb(pd_, "rs1d", [128, 1])
            tm1 = sb(pd_, "tm1d", [128, 1])
            osb = ust[0]
            phid = [ps(pd_, f"phid{i}", [128, 512]) for i in range(2)]
            py = [ps(pd_, f"py{i}", [128, 512]) for i in range(2)]
            pG = [ps(pd_, f"pG{i}", [128, 512]) for i in range(2)]
            put = ps(pd_, "put", [128, 4, 128])
            pat = ps(pd_, "pat", [128, 4, 128], BF16)
            NTG = 4
            chunk_box = [0]
            kk_box = [0]
            for tg in range(NTG):
                for j in range(4):
                    jj = tg * 4 + j
                    DMA(acc[:, j, :], hscr_d[jj * 128:(jj + 1) * 128, :])
                    DMA(s2t[:, j, :], gs2_d[jj])
                    DMA(cct[:, j, :], gc_d[jj])
                    DMA(gat[:, j, :], ga_d[jj])
                    DMA(gpt[:, j, :], gp_d[jj])
                DMA(hnT[:], hnscr_d.ap().rearrange("(c p) t -> p c t", p=128)[:, :, tg * 512:(tg + 1) * 512])
                NW = 128

                def load_eg(eg):
                    if tg > 0:
                        DMA(UT[:].rearrange("p a b -> p (a b)"), utscr_d[eg], chan='ldu')
                        DMA(Vb[eg % 2][:].rearrange("p a b -> p (a b)"), vscr_d[eg], chan='ldv')
                        return
                    for ic in range(4):
                        i = eg * 4 + ic
                        u_ = ust[chunk_box[0] % 2]
                        chunk_box[0] += 1
                        DMA(u_[:], pu_d[i * 128:(i + 1) * 128, :], chan='ldu')
                        DMA(vst[:], pv_d[i * 128:(i + 1) * 128, :], chan='ldv')
                        for d4 in range(4):
                            TRS([(put[:, k, :], u_[:, (d4 * 4 + k) * 128:(d4 * 4 + k + 1) * 128], ident) for k in range(4)])
                            CP(UT[:, d4 * 4:(d4 + 1) * 4, ic * 128:(ic + 1) * 128], put[:], eng='act')
                        CP(Vb[eg % 2][:, ic, :], vst[:], eng='pool')
                    DMA(utscr_d[eg], UT[:].rearrange("p a b -> p (a b)"), chan='st')
                    DMA(vscr_d[eg], Vb[eg % 2][:].rearrange("p a b -> p (a b)"), chan='st')

                def hid(w):
                    eg, j = divmod(w, 4)
                    ph = phid[w % 2]
                    MM(ph[:], [(hnT[:, dc, j * 128:(j + 1) * 128], UT[:, dc, :]) for dc in range(16)])
                    ACT(hg[w % 3][:], ph[:], AF.Gelu)

                def gate(w, hq):
                    eg, j = divmod(w, 4)
                    G = pG[w % 2]
                    for h in (2 * hq, 2 * hq + 1):
                        tb = tmpg[kk_box[0] % 8]
                        kk_box[0] += 1
                        for ic in range(4):
                            i = eg * 4 + ic
                            STT(tb[:, ic * 128:(ic + 1) * 128], s2t[:, j, h * 128:(h + 1) * 128],
                                cct[:, j, h * 128 + i:h * 128 + i + 1], gpt[:, j, h * 128:(h + 1) * 128], ALU.is_ge, ALU.mult)
                        for ic in range(4):
                            i = eg * 4 + ic
                            asc = gat[:, j, h * 128 + i:h * 128 + i + 1]
                            if ic % 2 == 0:
                                TS(tb[:, ic * 128:(ic + 1) * 128], tb[:, ic * 128:(ic + 1) * 128], asc, ALU.mult, 1.0, ALU.mult,
                                   eng='pool')
                            else:
                                ACT(tb[:, ic * 128:(ic + 1) * 128], tb[:, ic * 128:(ic + 1) * 128], AF.Copy, scale=asc)
                        MM(G[:], [(identb, tb[:])], first=(h == 0), last=(h == 7))

                def ttA(w):
                    A = Am[w % 2]
                    TT(A[:], hg[w % 3][:], pG[w % 2][:], ALU.mult)
                    TRS([(pat[:, ic, :], A[:, ic * 128:(ic + 1) * 128], identb) for ic in range(4)])
                    CP(AT[w % 2][:], pat[:], eng='act')

                def ymm(w, half):
                    eg, j = divmod(w, 4)
                    for n2 in range(2):
                        n = half * 2 + n2
                        MM(py[n2][:], [(AT[w % 2][:, ic, :], Vb[eg % 2][:, ic, n * 512:(n + 1) * 512]) for ic in range(4)])

                def flush(w, half):
                    j = w % 4
                    for n2 in range(2):
                        n = half * 2 + n2
                        TT(acc[:, j, n * 512:(n + 1) * 512], acc[:, j, n * 512:(n + 1) * 512], py[n2][:], ALU.add)

                load_eg(0)
                hid(0)
                for w in range(NW + 2):
                    if w + 1 < NW:
                        if (w + 1) % 4 == 0:
                            load_eg((w + 1) // 4)
                        hid(w + 1)
                    if w < NW:
                        gate(w, 0)
                    if 0 <= w - 2 < NW:
                        flush(w - 2, 0)
                        ymm(w - 2, 1)
                    if w < NW:
                        gate(w, 1)
                    if 0 <= w - 1 < NW:
                        ttA(w - 1)
                    if w < NW:
                        gate(w, 2)
                    if 0 <= w - 2 < NW:
                        flush(w - 2, 1)
                    if w < NW:
                        gate(w, 3)
                    if 0 <= w - 1 < NW:
                        ymm(w - 1, 0)
                for j in range(4):
                    jj = tg * 4 + j
                    ACT(osb[:], acc[:, j, :], AF.Square, accum=ss1[:])
                    rstd_from_ss(rs1[:], ss1[:], float(D), tm1[:])
                    STT(osb[:], acc[:, j, :], rs1[:], fing[:], ALU.mult, ALU.mult)
                    DMA(out_d[jj * 128:(jj + 1) * 128, :], osb[:], chan='out')
            tk.barrier()
        tk._wait('sp', {k: v for k, v in tk.dcnt.items()})
        print("instructions emitted:", tk.ninst)
    return nc, dbg


def _consts():
    c = np.zeros((128, 4, 128), np.float32)
    s = np.arange(128)[:, None]
    t = np.arange(128)[None, :]
    c[:, 0] = (s == t)
    c[:, 1] = (s <= t)
    c[:, 2] = (s < t)
    c[:, 3] = 1.0
    return c.reshape(128, 512)


def make_in_maps(x, attn_norm_g, w_in, hgrn_lb_logits, hgrn_norm_g, gdn_conv_w, gdn_A_log, gdn_dt_bias, gdn_norm_g,
                 w_out, ffn_norm_g, peer_w_query, peer_sub_keys, peer_u, peer_v, final_norm_g):
    f = lambda a: np.ascontiguousarray(np.asarray(a, dtype=np.float32))
    x = f(x)
    w_in0 = f(w_in)[0]
    lbl = f(hgrn_lb_logits)
    cwf = f(gdn_conv_w)[0]
    wo0 = f(w_out)[0]
    wq0 = f(peer_w_query)[0]
    sk0 = f(peer_sub_keys)[0]
    pu = f(peer_u)[0]
    pv = f(peer_v)[0]
    chunked = lambda v: np.ascontiguousarray(v.reshape(16, 128).T)
    ag = chunked(f(attn_norm_g)[0])
    fg = chunked(f(ffn_norm_g)[0])
    fin = np.ascontiguousarray(np.broadcast_to(f(final_norm_g)[None, :], (128, D)))
    hg = f(hgrn_norm_g)[0]
    gg = f(gdn_norm_g)[0]
    gain512 = np.ascontiguousarray(np.broadcast_to(np.concatenate([hg, hg, gg, gg])[None, :], (128, 512)))
    wq = np.ascontiguousarray(wq0.reshape(D, 8, 2, 128).transpose(0, 2, 1, 3).reshape(D, D))
    skt = np.ascontiguousarray(sk0.transpose(3, 0, 1, 2).reshape(128, 16 * 128))
    consts = _consts()
    maps = []
    for c in range(8):
        b, g = c // 4, c % 4
        cols = np.concatenate([
            np.arange(0 + 2 * g * 128, 0 + 2 * g * 128 + 256),
            np.arange(1024 + 2 * g * 128, 1024 + 2 * g * 128 + 256),
            np.arange(4096 + g * 128, 4096 + g * 128 + 128),
            np.arange(4608 + g * 128, 4608 + g * 128 + 128),
            np.arange(5120 + 2 * g * 128, 5120 + 2 * g * 128 + 256),
            np.arange(2048 + 2 * g * 128, 2048 + 2 * g * 128 + 256),
            np.arange(3072 + 2 * g * 128, 3072 + 2 * g * 128 + 256),
            np.arange(6144 + 2 * g * 128, 6144 + 2 * g * 128 + 256),
            np.arange(7168 + 2 * g, 7168 + 2 * g + 2),
            np.arange(7176 + 2 * g, 7176 + 2 * g + 2),
        ])
        wloc = np.ascontiguousarray(w_in0[:, cols])
        lb4 = np.ascontiguousarray(lbl[:, 2 * g:2 * g + 2, :].transpose(2, 0, 1).reshape(128, 4))
        cch = np.concatenate([np.arange(g * 128, g * 128 + 128), np.arange(512 + g * 128, 512 + g * 128 + 128),
                              np.arange(1024 + 2 * g * 128, 1024 + 2 * g * 128 + 256)])
        cwl = np.ascontiguousarray(cwf[:, cch].reshape(4, 4, 128).transpose(2, 1, 0).reshape(128, 16))
        al = np.ascontiguousarray(np.broadcast_to(f(gdn_A_log)[0, 2 * g:2 * g + 2][None, :], (128, 2)))
        dtb = np.ascontiguousarray(np.broadcast_to(f(gdn_dt_bias)[0, 2 * g:2 * g + 2][None, :], (128, 2)))
        maps.append({
            "x": x[b], "xs": np.ascontiguousarray(x[b, g * TOKB:(g + 1) * TOKB]),
            "w_in": wloc, "attn_g": ag, "lb_logits": lb4, "gain512": gain512, "conv_w": cwl,
            "a_log": al, "dt_bias": dtb, "w_out": None, "ffn_g": fg, "w_query": wq, "sk_t": skt,
            "peer_u": pu, "peer_v": pv, "final_g": fin, "consts": consts,
            "tok_off": np.array([[g * 8192]], np.int32),
        })
    rows = []
    for r in range(4):
        for q in range(4):
            base = (2 * r + q) * 128 if q < 2 else 1024 + (2 * r + q - 2) * 128
            rows.append(np.arange(base, base + 128))
    wop = np.ascontiguousarray(wo0[np.concatenate(rows), :])
    for m in maps:
        m["w_out"] = wop
    return maps


_CACHE = {}


def kernel(**inputs):
    if "nc" not in _CACHE:
        _CACHE["nc"] = build_program(debug=False)[0]
    nc = _CACHE["nc"]
    maps = make_in_maps(**inputs)
    res = run_bass_kernel_spmd(nc, maps, core_ids=list(range(8)))
    out = np.zeros((2, SEQ, D), np.float32)
    for c in range(8):
        b, g = c // 4, c % 4
        out[b, g * TOKB:(g + 1) * TOKB] = res.results[c]["out"]
    return out
```

```python
import numpy as np
from contextlib import ExitStack
import concourse.bass as bass
import concourse.mybir as mybir
from concourse.bass_utils import run_bass_kernel_spmd

F32 = mybir.dt.float32
BF16 = mybir.dt.bfloat16
I32 = mybir.dt.int32
AF = mybir.ActivationFunctionType
ALU = mybir.AluOpType

EPS = 1e-6
SEQ = 8192
D = 2048
NTA = 64
NST = 16
TOKB = 2048
NEG = -1.0e30

DEBUG = {}
PSUM_EXCL = True
NOSELF = False


class TK:
    def __init__(self, nc, es):
        self.nc = nc
        self.es = es
        self.eng = {'pe': nc.tensor, 'act': nc.scalar, 'dve': nc.vector, 'pool': nc.gpsimd, 'sp': nc.sync}
        self.sem = {k: es.enter_context(nc.semaphore("s_" + k)) for k in self.eng}
        self.cnt = {k: 0 for k in self.eng}
        self.waited = {k: {} for k in self.eng}
        self.dsem = {}
        self.dcnt = {}
        self.rings = {}
        self.lastw = {}
        self.readers = {}
        self.ninst = 0
        self.dead = False
        self.psum_names = set()

    @staticmethod
    def key(x):
        if isinstance(x, str):
            return x
        if hasattr(x, 'tensor'):
            return x.tensor.name
        return x.name

    @classmethod
    def keys(cls, x):
        k = cls.key(x)
        if k.startswith("tmpg") and hasattr(x, 'shape'):
            if x.shape[-1] == 128:
                return [f"{k}:{(x.offset % 512) // 128}"]
            return [f"{k}:{i}" for i in range(4)]
        return [k]

    def _wait(self, e, deps):
        if self.dead:
            return
        for k, v in deps.items():
            if k == e and (e == 'pe' or NOSELF):
                continue
            if self.waited[e].get(k, 0) >= v:
                continue
            sem = self.sem[k] if k in self.sem else self.dsem[k]
            self.eng[e].wait_ge(sem, v)
            self.waited[e][k] = v
            self.ninst += 1

    def _collect(self, reads, writes):
        deps = {}

        def add(tok):
            if tok is not None:
                deps[tok[0]] = max(deps.get(tok[0], 0), tok[1])
        for r in reads:
            for kr in self.keys(r):
                add(self.lastw.get(kr))
        for w in writes:
            for kw in self.keys(w):
                add(self.lastw.get(kw))
                for k, v in self.readers.get(kw, {}).items():
                    add((k, v))
        return deps

    def _record(self, tok, reads, writes):
        for r in reads:
            for kr in self.keys(r):
                d = self.readers.setdefault(kr, {})
                d[tok[0]] = max(d.get(tok[0], 0), tok[1])
        for w in writes:
            for kw in self.keys(w):
                self.lastw[kw] = tok
                self.readers[kw] = {}

    def op(self, e, fns, reads=(), writes=()):
        if self.dead:
            return None
        if PSUM_EXCL:
            ex = [r for r in reads if self.key(r) in self.psum_names]
            if ex:
                writes = list(writes) + ex
        if not isinstance(fns, (list, tuple)):
            fns = [fns]
        deps = self._collect(reads, writes)
        self._wait(e, deps)
        inst = None
        for f in fns:
            inst = f(self.eng[e])
            self.ninst += 1
        self.cnt[e] += 1
        inst.then_inc(self.sem[e], 1)
        self._record((e, self.cnt[e]), reads, writes)
        return inst

    def dma(self, q, chan, out, in_, extra_reads=(), extra_writes=(), **kw):
        if self.dead:
            return None
        ring = self.rings.setdefault(q, {'n': 40 if q == 'sp' else 8, 'i': 0})
        name = f"{q}{ring['i'] % ring['n']}"
        ring['i'] += 1
        if name not in self.dsem:
            self.dsem[name] = self.es.enter_context(self.nc.semaphore("d_" + name))
            self.dcnt[name] = 0
        reads = [in_] + list(extra_reads)
        writes = [out] + list(extra_writes)
        deps = self._collect(reads, writes)
        if self.dcnt[name] > 0:
            deps[name] = max(deps.get(name, 0), self.dcnt[name])
        self._wait(q, deps)
        inst = self.eng[q].dma_start(out=out, in_=in_, **kw)
        self.ninst += 1
        self.dcnt[name] += 16
        inst.then_inc(self.dsem[name], 16)
        self._record((name, self.dcnt[name]), reads, writes)
        return inst

    def barrier(self):
        allc = {k: v for k, v in self.cnt.items() if v > 0}
        allc.update({k: v for k, v in self.dcnt.items() if v > 0})
        for e in self.eng:
            self._wait(e, {k: v for k, v in allc.items() if k != e})


def build_program(debug=False, nst=NST, stop=''):
    nc = bass.Bass("TRN2", target_bir_lowering=False)

    def din(name, shape, dt=F32):
        if stop and stop[0] != 'B' and name in ("xs", "w_out", "w_query", "sk_t", "peer_u", "peer_v", "final_g"):
            return None
        if stop in ('B0', 'B1', 'B2') and name in ("peer_u", "peer_v", "final_g"):
            return None
        return nc.dram_tensor(name, list(shape), dt, kind="ExternalInput")

    x_d = din("x", [SEQ, D])
    xs_d = din("xs", [TOKB, D])
    win_d = din("w_in", [D, 1796])
    ag_d = din("attn_g", [128, 16])
    lbl_d = din("lb_logits", [128, 4])
    gain_d = din("gain512", [128, 512])
    cw_d = din("conv_w", [128, 16])
    al_d = din("a_log", [128, 2])
    dtb_d = din("dt_bias", [128, 2])
    wo_d = din("w_out", [D, D])
    fg_d = din("ffn_g", [128, 16])
    wq_d = din("w_query", [D, D])
    sk_d = din("sk_t", [128, 16 * 128])
    pu_d = din("peer_u", [16384, D])
    pv_d = din("peer_v", [16384, D])
    fin_d = din("final_g", [128, D])
    cst_d = din("consts", [128, 4 * 128])
    off_d = din("tok_off", [1, 1], I32)
    out_d = nc.dram_tensor("out", [TOKB, D], F32, kind="ExternalOutput")

    cin_ds = [nc.dram_tensor(f"cin{k}", [1024, 512], BF16) for k in range(8)]
    cout_d = nc.dram_tensor("cout", [8 * 4096 + 8192, 512], BF16)
    hscr_d = nc.dram_tensor("hscr", [TOKB, D], F32)
    hnscr_d = nc.dram_tensor("hnscr", [D, TOKB], BF16)
    gs2_d = nc.dram_tensor("gs2", [16, 128, 1024], F32)
    gc_d = nc.dram_tensor("gcc", [16, 128, 1024], F32)
    ga_d = nc.dram_tensor("gaa", [16, 128, 1024], F32)
    gp_d = nc.dram_tensor("gpp", [16, 128, 1024], BF16)
    utscr_d = nc.dram_tensor("utscr", [32, 128, 8192], BF16)
    vscr_d = nc.dram_tensor("vscr", [32, 128, 8192], BF16)

    dbg = {}

    def dout(name, shape, dt=F32):
        t = nc.dram_tensor(name, list(shape), dt, kind="ExternalOutput")
        dbg[name] = t
        return t

    with ExitStack() as top:
        tk = TK(nc, top)
        keep = []

        def chk(tag):
            if stop == tag and not tk.dead:
                tk.barrier()
                tk.dead = True

        def sb(es, name, shape, dt=F32):
            t = es.enter_context(nc.sbuf_tensor(name, list(shape), dt))
            keep.append(t)
            return t

        def ps(es, name, shape, dt=F32):
            t = es.enter_context(nc.psum_tensor(name, list(shape), dt))
            tk.psum_names.add(name)
            keep.append(t)
            return t

        def aps(*xs):
            return [a for a in xs if hasattr(a, 'tensor')]

        def ACT(out, in_, func, bias=0.0, scale=1.0, accum=None, eng='act'):
            kw = {}
            if accum is not None:
                kw['accum_out'] = accum
            tk.op('act', lambda e: e.activation(out=out, in_=in_, func=func, bias=bias, scale=scale, **kw),
                  reads=aps(in_, bias, scale), writes=aps(out, accum))

        def TS(out, in0, s1, op0, s2=None, op1=None, eng='dve'):
            if op1 is None:
                tk.op(eng, lambda e: e.tensor_scalar(out=out, in0=in0, scalar1=s1, scalar2=None, op0=op0),
                      reads=aps(in0, s1), writes=aps(out))
            else:
                tk.op(eng, lambda e: e.tensor_scalar(out=out, in0=in0, scalar1=s1, scalar2=s2, op0=op0, op1=op1),
                      reads=aps(in0, s1, s2), writes=aps(out))

        def TT(out, in0, in1, op, eng='dve'):
            tk.op(eng, lambda e: e.tensor_tensor(out=out, in0=in0, in1=in1, op=op), reads=aps(in0, in1), writes=aps(out))

        def STT(out, in0, scalar, in1, op0, op1):
            tk.op('dve', lambda e: e.scalar_tensor_tensor(out=out, in0=in0, scalar=scalar, in1=in1, op0=op0, op1=op1),
                  reads=aps(in0, scalar, in1), writes=aps(out))

        def CP(out, in_, eng='dve'):
            if eng == 'act':
                tk.op('act', lambda e: e.activation(out=out, in_=in_, func=AF.Copy), reads=aps(in_), writes=aps(out))
            else:
                tk.op(eng, lambda e: e.tensor_copy(out=out, in_=in_), reads=aps(in_), writes=aps(out))

        def RECIP(out, in_):
            tk.op('dve', lambda e: e.reciprocal(out=out, in_=in_), reads=aps(in_), writes=aps(out))

        def MM(out, pairs, extra_reads=(), first=True, last=True):
            n = len(pairs)
            fns = []
            rd = list(extra_reads)
            for i, (l, r) in enumerate(pairs):
                fns.append(lambda e, l=l, r=r, i=i: e.matmul(out, lhsT=l, rhs=r, start=(first and i == 0),
                                                             stop=(last and i == n - 1)))
                rd += [l, r]
            tk.op('pe', fns, reads=rd, writes=[out])

        def TRS(items):
            fns = []
            rd = []
            wr = []
            for (o, i, idn) in items:
                fns.append(lambda e, o=o, i=i, idn=idn: e.transpose(out=o, in_=i, identity=idn))
                rd += [i, idn]
                wr.append(o)
            tk.op('pe', fns, reads=rd, writes=wr)

        def DMA(out, in_, q='sp', chan='ld', **kw):
            tk.dma(q, chan, out, in_, **kw)

        def interleave2(*lists):
            n = max(len(l) for l in lists)
            for k in range(n):
                for l in lists:
                    if k < len(l):
                        l[k]()

        def rstd_from_ss(rs, ss, n, tmp):
            TS(tmp, ss, 1.0 / n, ALU.mult, EPS, ALU.add)
            ACT(tmp, tmp, AF.Sqrt)
            RECIP(rs, tmp)

        cst = sb(top, "cst", [128, 4, 128])
        cstb = sb(top, "cstb", [128, 4, 128], BF16)
        DMA(cst[:].rearrange("p a b -> p (a b)"), cst_d[:, :])
        CP(cstb[:], cst[:])
        ident, maskU, strictU, ones = (cst[:, i, :] for i in range(4))
        identb, maskUb, strictUb, onesb = (cstb[:, i, :] for i in range(4))

        with ExitStack() as pa:
            wsb = sb(pa, "wsb", [128, 16, 1796], BF16)
            agsb = sb(pa, "agsb", [128, 16])
            DMA(agsb[:], ag_d[:, :])
            with ExitStack() as tmpes:
                wst = [sb(tmpes, f"wst{i}", [128, 1796]) for i in range(2)]
                for dc in range(16):
                    DMA(wst[dc % 2][:], win_d[dc * 128:(dc + 1) * 128, :])
                    TS(wsb[:, dc, :], wst[dc % 2][:], agsb[:, dc:dc + 1], ALU.mult, eng=('dve' if dc % 2 == 0 else 'pool'))
                tk.barrier()
            lbl = sb(pa, "lbl", [128, 4])
            oml = sb(pa, "oml", [128, 2])
            gain = sb(pa, "gain", [128, 512])
            cw = sb(pa, "cw", [128, 4, 4])
            nexpA = sb(pa, "nexpA", [128, 2])
            dtb = sb(pa, "dtb", [128, 2])
            DMA(lbl[:], lbl_d[:, :])
            DMA(gain[:], gain_d[:, :])
            DMA(cw[:].rearrange("p a b -> p (a b)"), cw_d[:, :])
            DMA(nexpA[:], al_d[:, :])
            DMA(dtb[:], dtb_d[:, :])
            TT(oml[:], lbl[:, 2:4], lbl[:, 0:2], ALU.subtract)
            ACT(oml[:], oml[:], AF.Sigmoid)
            ACT(nexpA[:], nexpA[:], AF.Exp)
            TS(nexpA[:], nexpA[:], -1.0, ALU.mult)
            onesf = sb(pa, "onesf", [128, 128])
            tk.op('pool', lambda e: e.memset(onesf[:], 1.0), writes=[onesf])

            xt = [sb(pa, f"xt{i}", [128, D]) for i in range(2)]
            xb = sb(pa, "xb", [128, D], BF16)
            ss1 = sb(pa, "ss1", [128, 1])
            rs1 = sb(pa, "rs1", [128, 1])
            tm1 = sb(pa, "tm1", [128, 1])
            xnT = sb(pa, "xnT", [128, 16, 512], BF16)
            qT = [sb(pa, f"qT{h}", [128, 512]) for h in range(2)]
            kT = [sb(pa, f"kT{h}", [128, 512]) for h in range(2)]
            lgf = [sb(pa, f"lgf{h}", [128, 512]) for h in range(2)]
            bT = [sb(pa, f"bT{h}", [128, 512]) for h in range(2)]
            etmp = sb(pa, "etmp", [128, 512])
            qeT = [sb(pa, f"qeT{h}", [128, 512], BF16) for h in range(2)]
            kbT = [sb(pa, f"kbT{h}", [128, 512], BF16) for h in range(2)]
            kdT = [sb(pa, f"kdT{h}", [128, 512], BF16) for h in range(2)]
            dec = [sb(pa, f"dec{h}", [128, 4]) for h in range(2)]
            cb = [sb(pa, f"cb{i}", [128, 515]) for i in range(4)]
            cacc = sb(pa, "cacc", [128, 512])
            csl = [sb(pa, f"csl{i}", [128, 512]) for i in range(2)]
            sqb = sb(pa, "sqb", [128, 512], BF16)
            rn = sb(pa, "rn", [128, 512])
            qnT = sb(pa, "qnT", [128, 512], BF16)
            knT = sb(pa, "knT", [128, 512], BF16)
            vT = [sb(pa, f"vT{i}", [128, 512], BF16) for i in range(2)]
            vA = [sb(pa, f"vA{j}", [128, 256], BF16) for j in range(4)]
            gzs = [sb(pa, f"gzs{j}", [128, 512]) for j in range(4)]
            bba = [sb(pa, f"bba{j}", [128, 4]) for j in range(4)]
            SA = [sb(pa, f"SA{h}", [128, 128]) for h in range(2)]
            SAb = [sb(pa, f"SAb{h}", [128, 128], BF16) for h in range(2)]
            SB = [sb(pa, f"SB{h}", [128, 128]) for h in range(2)]
            SBb = [sb(pa, f"SBb{h}", [128, 128], BF16) for h in range(2)]
            for t in SA + SB + SAb + SBb:
                tk.op('pool', lambda e, t=t: e.memset(t[:], 0.0), writes=[t])
            for i in range(4):
                tk.op('pool', lambda e, i=i: e.memset(cb[i][:, 0:3], 0.0), writes=[cb[i]])
            attb2 = [[sb(pa, f"attb{h}{p}", [128, 128], BF16) for p in range(2)] for h in range(2)]
            kdb2 = [[sb(pa, f"kdb{h}{p}", [128, 128], BF16) for p in range(2)] for h in range(2)]
            oall = sb(pa, "oall", [128, 512])
            ss4 = sb(pa, "ss4", [128, 4])
            rs4 = sb(pa, "rs4", [128, 4])
            tm4 = sb(pa, "tm4", [128, 4])
            junk = sb(pa, "junk", [128, 128])
            mixt = [sb(pa, f"mixt{i}", [128, 512], BF16) for i in range(2)]
            KKm = sb(pa, "KKm", [128, 128])
            QKm = sb(pa, "QKm", [128, 128])
            g2 = sb(pa, "g2", [128, 2])
            beta2 = sb(pa, "beta2", [128, 2])
            sp2 = sb(pa, "sp2", [128, 2])
            gcs = sb(pa, "gcs", [128, 2])
            gls = sb(pa, "gls", [128, 2])
            egc2 = [sb(pa, f"egc{p}", [128, 2]) for p in range(2)]
            ekd = sb(pa, "ekd", [128, 2])
            dl2 = [sb(pa, f"dl{p}", [128, 2]) for p in range(2)]
            bge = sb(pa, "bge", [128, 2])
            dg = [sb(pa, f"dg{v}", [128, 256]) for v in range(2)]
            Dm = [sb(pa, f"Dm{v}", [128, 128]) for v in range(2)]
            Ee = [sb(pa, f"Ee{v}", [128, 128]) for v in range(2)]
            aqk2 = [[sb(pa, f"aqk{v}{p}", [128, 128], BF16) for p in range(2)] for v in range(2)]
            Xk = [sb(pa, f"Xk{v}", [128, 128]) for v in range(2)]
            Pa = [[sb(pa, f"Pa{v}{i}", [128, 128]) for i in range(2)] for v in range(2)]
            Pt = [[sb(pa, f"Pt{v}{i}", [128, 128]) for i in range(2)] for v in range(2)]
            Tt = [[sb(pa, f"Tt{v}{i}", [128, 128]) for i in range(2)] for v in range(2)]
            Ttb = [sb(pa, f"Ttb{v}", [128, 128], BF16) for v in range(2)]
            vbt = [sb(pa, f"vbt{v}", [128, 128], BF16) for v in range(2)]
            kbg = [sb(pa, f"kbg{v}", [128, 128], BF16) for v in range(2)]
            kdg2 = [[sb(pa, f"kdg{v}{p}", [128, 128], BF16) for p in range(2)] for v in range(2)]
            us2 = [[sb(pa, f"us{v}{p}", [128, 128]) for p in range(2)] for v in range(2)]
            wTb2 = [[sb(pa, f"wTb{v}{p}", [128, 128], BF16) for p in range(2)] for v in range(2)]
            vnew = [sb(pa, f"vnew{v}", [128, 128], BF16) for v in range(2)]
            o1 = [sb(pa, f"o1{v}", [128, 128]) for v in range(2)]

            ptr = [ps(pa, f"ptr{i}", [128, 1024], BF16) for i in range(2)]
            pbig = [ps(pa, f"pbig{i}", [128, 512]) for i in range(3)]
            psmb = [ps(pa, f"psm{i}", [128, 4, 128]) for i in range(3)]
            psm_i = [0]

            class Sub:
                def __init__(self, ap):
                    self.ap = ap

                def __getitem__(self, idx):
                    assert idx == slice(None)
                    return self.ap

            def small():
                k = psm_i[0] % 12
                psm_i[0] += 1
                return Sub(psmb[k % 3][:, k // 3, :])
            pbig_i = [0]

            def big():
                t = pbig[pbig_i[0] % 3]
                pbig_i[0] += 1
                return t

            for st in range(nst):
                for j in range(4):
                    tt = st * 4 + j
                    xtile = xt[tt % 2]
                    DMA(xtile[:], x_d[tt * 128:(tt + 1) * 128, :])
                    ACT(xb[:], xtile[:], AF.Square, accum=ss1[:])
                    rstd_from_ss(rs1[:], ss1[:], float(D), tm1[:])
                    ACT(xb[:], xtile[:], AF.Copy, scale=rs1[:])
                    for hf in range(2):
                        TRS([(ptr[hf][:, i * 128:(i + 1) * 128], xb[:, (hf * 8 + i) * 128:(hf * 8 + i + 1) * 128], identb)
                             for i in range(8)])
                        CP(xnT[:, hf * 8:(hf + 1) * 8, j * 128:(j + 1) * 128],
                           ptr[hf][:].rearrange("p (a b) -> p a b", b=128), eng=('dve' if hf == 0 else 'act'))
                chk('A1')
                for blk in range(8):
                    pb = big()
                    MM(pb[:], [(wsb[:, dc, blk * 128:(blk + 1) * 128], xnT[:, dc, :]) for dc in range(16)])
                    if blk < 2:
                        CP(qT[blk][:], pb[:], eng='act')
                    elif blk < 4:
                        h = blk - 2
                        ACT(kT[h][:], pb[:], AF.Sigmoid, scale=-1.0)
                        TS(kT[h][:], kT[h][:], oml[:, h:h + 1], ALU.mult)
                        ACT(lgf[h][:], kT[h][:], AF.Ln, scale=-1.0, bias=1.0)
                    else:
                        CP(cb[blk - 4][:, 3:515], pb[:], eng='act')
                chk('A2')
                for j in range(4):
                    pb = big()
                    MM(pb[:], [(xnT[:, dc, j * 128:(j + 1) * 128], wsb[:, dc, 1024:1536]) for dc in range(16)])
                    CP(vA[j][:], pb[:, 0:256])
                    ACT(gzs[j][:, 0:256], pb[:, 256:512], AF.Silu)
                    pb2 = big()
                    MM(pb2[:, 0:260], [(xnT[:, dc, j * 128:(j + 1) * 128], wsb[:, dc, 1536:1796]) for dc in range(16)])
                    ACT(gzs[j][:, 256:512], pb2[:, 0:256], AF.Silu)
                    CP(bba[j][:], pb2[:, 256:260])
                    TT(gzs[j][:], gzs[j][:], gain[:], ALU.mult)
                chk('A3')
                for h in range(2):
                    for c in range(4):
                        sl = slice(c * 128, (c + 1) * 128)
                        tk.op('dve', lambda e, h=h, sl=sl: e.tensor_tensor_scan(
                            out=bT[h][:, sl], data0=onesf[:], data1=lgf[h][:, sl], initial=0.0,
                            op0=ALU.mult, op1=ALU.add), reads=[onesf, lgf[h]], writes=[bT[h]])
                    ACT(etmp[:], bT[h][:], AF.Exp)
                    TT(qeT[h][:], qT[h][:], etmp[:], ALU.mult)
                    ACT(etmp[:], bT[h][:], AF.Exp, scale=-1.0)
                    TT(kbT[h][:], kT[h][:], etmp[:], ALU.mult)
                    for c in range(4):
                        sl = slice(c * 128, (c + 1) * 128)
                        ACT(etmp[:, sl], bT[h][:, sl], AF.Exp, scale=-1.0, bias=bT[h][:, c * 128 + 127:c * 128 + 128])
                        ACT(dec[h][:, c:c + 1], bT[h][:, c * 128 + 127:c * 128 + 128], AF.Exp)
                    TT(kdT[h][:], kT[h][:], etmp[:], ALU.mult)
                chk('A4')
                for i in range(4):
                    TS(cacc[:], cb[i][:, 3:515], cw[:, i, 3:4], ALU.mult)
                    for tap in range(3):
                        STT(cacc[:], cb[i][:, tap:tap + 512], cw[:, i, tap:tap + 1], cacc[:], ALU.mult, ALU.add)
                    CP(cb[i][:, 0:3], cb[i][:, 512:515], eng='pool')
                    if i < 2:
                        ACT(csl[i][:], cacc[:], AF.Silu)
                        ACT(sqb[:], csl[i][:], AF.Square)
                        pb = big()
                        MM(pb[:], [(onesb, sqb[:])])
                        ACT(rn[:], pb[:], AF.Sqrt, bias=EPS)
                        RECIP(rn[:], rn[:])
                        if i == 0:
                            STT(qnT[:], csl[0][:], 128.0 ** -0.5, rn[:], ALU.mult, ALU.mult)
                        else:
                            TT(knT[:], csl[1][:], rn[:], ALU.mult)
                    else:
                        ACT(vT[i - 2][:], cacc[:], AF.Silu)

                chk('A5')
                def prepH(c):
                    sl = slice(c * 128, (c + 1) * 128)
                    p = c % 2

                    def ops(h):
                        pa_ = small()
                        pk = ptr[0]
                        return [
                            lambda: MM(pa_[:], [(kbT[h][:, sl], qeT[h][:, sl])]),
                            lambda: TT(attb2[h][p][:], pa_[:], maskU, ALU.mult),
                            lambda: TRS([(pk[:, h * 128:(h + 1) * 128], kdT[h][:, sl], identb)]),
                            lambda: CP(kdb2[h][p][:], pk[:, h * 128:(h + 1) * 128], eng='act'),
                        ]
                    interleave2(ops(0), ops(1))

                def seqH(c):
                    sl = slice(c * 128, (c + 1) * 128)
                    p = c % 2

                    def ops(h):
                        po = small()
                        p4 = small()
                        return [
                            lambda: MM(po[:], [(qeT[h][:, sl], SAb[h][:]), (attb2[h][p][:], vA[c][:, h * 128:(h + 1) * 128])]),
                            lambda: MM(p4[:], [(kdb2[h][p][:], vA[c][:, h * 128:(h + 1) * 128])]),
                            lambda: STT(SA[h][:], SA[h][:], dec[h][:, c:c + 1], p4[:], ALU.mult, ALU.add),
                            lambda: CP(oall[:, h * 128:(h + 1) * 128], po[:], eng='act'),
                            lambda: CP(SAb[h][:], SA[h][:], eng='act'),
                        ]
                    interleave2(ops(0), ops(1))

                def prepG(c):
                    sl = slice(c * 128, (c + 1) * 128)
                    p = c % 2
                    egc, dl = egc2[p], dl2[p]
                    ACT(beta2[:], bba[c][:, 0:2], AF.Sigmoid)
                    TT(sp2[:], bba[c][:, 2:4], dtb[:], ALU.add)
                    ACT(sp2[:], sp2[:], AF.Exp)
                    ACT(sp2[:], sp2[:], AF.Ln, bias=1.0)
                    TT(g2[:], sp2[:], nexpA[:], ALU.mult)
                    pg = big()
                    MM(pg[:, 0:2], [(maskU, g2[:])])
                    MM(pg[:, 2:4], [(ones, g2[:])])
                    CP(gcs[:], pg[:, 0:2])
                    CP(gls[:], pg[:, 2:4])
                    ACT(egc[:], gcs[:], AF.Exp)
                    ACT(dl[:], gls[:], AF.Exp)
                    TT(ekd[:], gls[:], gcs[:], ALU.subtract)
                    ACT(ekd[:], ekd[:], AF.Exp)
                    TT(bge[:], beta2[:], egc[:], ALU.mult)
                    pkk = small()
                    MM(pkk[:], [(knT[:, sl], knT[:, sl])])
                    TT(KKm[:], pkk[:], strictU, ALU.mult)
                    pqk = small()
                    MM(pqk[:], [(knT[:, sl], qnT[:, sl])])
                    TT(QKm[:], pqk[:], maskU, ALU.mult)
                    pkt = ptr[1]
                    TRS([(pkt[:, 0:128], knT[:, sl], identb),
                         (pkt[:, 128:256], vT[0][:, sl], identb),
                         (pkt[:, 256:384], vT[1][:, sl], identb)])
                    for v in range(2):
                        TS(vbt[v][:], pkt[:, 128 * (v + 1):128 * (v + 2)], beta2[:, v:v + 1], ALU.mult)
                        TS(kbg[v][:], pkt[:, 0:128], bge[:, v:v + 1], ALU.mult)
                        TS(kdg2[v][p][:], pkt[:, 0:128], ekd[:, v:v + 1], ALU.mult)
                    for v in range(2):
                        TS(dg[v][:, 0:128], ident, gcs[:, v:v + 1], ALU.mult, eng='pool')
                        TS(dg[v][:, 128:256], ident, beta2[:, v:v + 1], ALU.mult, eng='pool')
                        prow = big()
                        MM(prow[:, 0:256], [(ones, dg[v][:])])
                        TS(Dm[v][:], prow[:, 0:128], gcs[:, v:v + 1], ALU.subtract, 0.0, ALU.min)
                        ACT(Ee[v][:], Dm[v][:], AF.Exp)
                        TT(aqk2[v][p][:], QKm[:], Ee[v][:], ALU.mult)
                        TT(Xk[v][:], KKm[:], Ee[v][:], ALU.mult)
                        TT(Pa[v][0][:], Xk[v][:], prow[:, 128:256], ALU.mult)
                        pT0 = small()
                        TRS([(pT0[:], Pa[v][0][:], ident)])
                        CP(Pt[v][0][:], pT0[:], eng='act')
                        TT(Tt[v][0][:], ident, Pa[v][0][:], ALU.subtract)
                    cur = 0
                    for lev in range(6):
                        nxt = 1 - cur
                        for v in range(2):
                            last = (lev == 5)
                            pPt = small()
                            MM(pPt[:], [(Pa[v][cur][:], Pt[v][cur][:])])
                            CP(Pt[v][nxt][:], pPt[:], eng='act')
                            if not last:
                                pP = small()
                                MM(pP[:], [(Pt[v][cur][:], Pa[v][cur][:])])
                                CP(Pa[v][nxt][:], pP[:], eng='act')
                            pTT = small()
                            MM(pTT[:], [(Pt[v][nxt][:], Tt[v][cur][:])])
                            if not last:
                                TT(Tt[v][nxt][:], Tt[v][cur][:], pTT[:], ALU.add)
                            else:
                                TT(Ttb[v][:], Tt[v][cur][:], pTT[:], ALU.add)
                        cur = nxt
                    for v in range(2):
                        pu = small()
                        MM(pu[:], [(Ttb[v][:], vbt[v][:])])
                        CP(us2[v][p][:], pu[:], eng='act')
                        pw = small()
                        MM(pw[:], [(kbg[v][:], Ttb[v][:])])
                        CP(wTb2[v][p][:], pw[:], eng='act')

                def seqG(c):
                    sl = slice(c * 128, (c + 1) * 128)
                    p = c % 2
                    egc, dl = egc2[p], dl2[p]

                    def ops(v):
                        p1, p2, p3, p4 = small(), small(), small(), small()
                        return [
                            lambda: MM(p1[:], [(wTb2[v][p][:], SBb[v][:])]),
                            lambda: MM(p2[:], [(qnT[:, sl], SBb[v][:])]),
                            lambda: TT(vnew[v][:], us2[v][p][:], p1[:], ALU.subtract),
                            lambda: TS(o1[v][:], p2[:], egc[:, v:v + 1], ALU.mult),
                            lambda: MM(p4[:], [(kdg2[v][p][:], vnew[v][:])]),
                            lambda: MM(p3[:], [(aqk2[v][p][:], vnew[v][:])]),
                            lambda: STT(SB[v][:], SB[v][:], dl[:, v:v + 1], p4[:], ALU.mult, ALU.add),
                            lambda: CP(SBb[v][:], SB[v][:], eng='act'),
                            lambda: TT(oall[:, (2 + v) * 128:(3 + v) * 128], o1[v][:], p3[:], ALU.add),
                        ]
                    interleave2(ops(0), ops(1))

                def fin(c):
                    tt = st * 4 + c
                    for hh in range(4):
                        ACT(junk[:], oall[:, hh * 128:(hh + 1) * 128], AF.Square, accum=ss4[:, hh:hh + 1])
                    rstd_from_ss(rs4[:], ss4[:], 128.0, tm4[:])
                    mt = mixt[tt % 2]
                    for hh in range(4):
                        STT(mt[:, hh * 128:(hh + 1) * 128], oall[:, hh * 128:(hh + 1) * 128], rs4[:, hh:hh + 1],
                            gzs[c][:, hh * 128:(hh + 1) * 128], ALU.mult, ALU.mult)
                    DMA(cin_ds[tt // 8][(tt % 8) * 128:(tt % 8 + 1) * 128, :], mt[:], chan='cin')
                    if debug and tt < 2:
                        od = dout(f"dbg_oall{tt}", [128, 512])
                        DMA(od[:, :], oall[:], chan='dbg')

                prepH(0)
                prepG(0)
                for c in range(4):
                    if c + 1 < 4:
                        prepH(c + 1)
                    seqH(c)
                    if c + 1 < 4:
                        prepG(c + 1)
                    seqG(c)
                    fin(c)
            tk.barrier()
        if stop and stop[0] != 'B':
            tk.dead = False
            od = dout("dbg_mix", [nst * 512, 512], BF16)
            for k in range((nst + 1) // 2):
                nr = min(1024, nst * 512 - k * 1024)
                tk.dma('sp', 'dbg', od[k * 1024:k * 1024 + nr, :], cin_ds[k][0:nr, :])
            tk._wait('sp', {k: v for k, v in tk.dcnt.items()})
            return nc, dbg

        if not tk.dead:
            for k in range(8):
                ccs = top.enter_context(nc.semaphore(f"ccs{k}"))
                nc.gpsimd.collective_compute("AllGather", ALU.bypass, replica_groups=[[0, 1, 2, 3], [4, 5, 6, 7]],
                                             ins=[cin_ds[k].ap().opt()],
                                             outs=[cout_d.ap()[k * 4096:(k + 1) * 4096, :].opt()]).then_inc(ccs)
                nc.gpsimd.wait_ge(ccs, 1)
        offt = sb(top, "offt", [1, 1], I32)
        DMA(offt[:], off_d[:, :], q='pool', chan='pl')
        tk._wait('pool', {k: v for k, v in tk.dcnt.items()})
        reg = top.enter_context(nc.gpsimd.register("roff"))
        if not tk.dead:
            nc.gpsimd.reg_load(reg, offt[0:1, 0:1])
        ov = nc.gpsimd.snap(reg)
        def cview(j):
            s0 = (j // 8) * 4096 + (j % 8) * 128
            R = cout_d.ap()[s0:s0 + 28672, :]
            return R[bass.ds(ov, 4096), :].rearrange("(r t) n -> t r n", r=4)[0:128, :, :]
        if stop == 'B0':
            od = dout("dbg_cout", [4 * 512, 512], BF16)
            tk.dma('sp', 'dbg', od[:, :], cout_d[0:2048, :])
            tk._wait('sp', {k: v for k, v in tk.dcnt.items()})
            return nc, dbg

        with ExitStack() as pb_:
            wo = sb(pb_, "wo", [128, 16, D], BF16)
            fgs = sb(pb_, "fgs", [128, 16])
            DMA(fgs[:], fg_d[:, :])
            with ExitStack() as tmpes:
                wst = [sb(tmpes, f"wost{i}", [128, D]) for i in range(2)]
                for cc in range(16):
                    DMA(wst[cc % 2][:], wo_d[cc * 128:(cc + 1) * 128, :])
                    CP(wo[:, cc, :], wst[cc % 2][:], eng=('act' if cc % 2 == 0 else 'pool'))
                tk.barrier()
            mixg = [sb(pb_, f"mixg{i}", [128, 4, 512], BF16) for i in range(2)]
            mixT = sb(pb_, "mixT", [128, 16, 128], BF16)
            xres = [sb(pb_, f"xres{i}", [128, D]) for i in range(2)]
            hsb = [sb(pb_, f"hsb{i}", [128, D]) for i in range(2)]
            hb = sb(pb_, "hb", [128, D], BF16)
            ss1 = sb(pb_, "ss1b", [128, 1])
            rs1 = sb(pb_, "rs1b", [128, 1])
            tm1 = sb(pb_, "tm1b", [128, 1])
            hnTj = [sb(pb_, f"hnTj{i}", [128, 16, 128], BF16) for i in range(2)]
            ptr = [ps(pb_, f"ptrb{i}", [128, 1024], BF16) for i in range(2)]
            po = [ps(pb_, f"pob{i}", [128, 512]) for i in range(4)]
            for j in range(16):
                mg = mixg[j % 2]
                tk.dma('pool', 'pl', mg[:], cview(j), extra_reads=[cout_d])
                DMA(xres[j % 2][:], xs_d[j * 128:(j + 1) * 128, :])
                for hf in range(2):
                    TRS([(ptr[hf][:, i * 128:(i + 1) * 128], mg[:, hf * 2 + i // 4, (i % 4) * 128:(i % 4 + 1) * 128], identb)
                         for i in range(8)])
                    CP(mixT[:, hf * 8:(hf + 1) * 8, :], ptr[hf][:].rearrange("p (a b) -> p a b", b=128),
                       eng=('dve' if hf == 0 else 'act'))
                hs_ = hsb[j % 2]
                for n in range(4):
                    MM(po[n][:], [(mixT[:, cc, :], wo[:, cc, n * 512:(n + 1) * 512]) for cc in range(16)])
                    TT(hs_[:, n * 512:(n + 1) * 512], po[n][:], xres[j % 2][:, n * 512:(n + 1) * 512], ALU.add)
                DMA(hscr_d[j * 128:(j + 1) * 128, :], hs_[:], chan='st')
                ACT(hb[:], hs_[:], AF.Square, accum=ss1[:])
                rstd_from_ss(rs1[:], ss1[:], float(D), tm1[:])
                ACT(hb[:], hs_[:], AF.Copy, scale=rs1[:])
                hT = hnTj[j % 2]
                for hf in range(2):
                    TRS([(ptr[hf][:, i * 128:(i + 1) * 128], hb[:, (hf * 8 + i) * 128:(hf * 8 + i + 1) * 128], identb)
                         for i in range(8)])
                    for i in range(8):
                        dc = hf * 8 + i
                        TS(hT[:, dc, :], ptr[hf][:, i * 128:(i + 1) * 128], fgs[:, dc:dc + 1], ALU.mult,
                           eng='dve')
                DMA(hnscr_d.ap().rearrange("(c p) t -> p c t", p=128)[:, :, j * 128:(j + 1) * 128], hT[:], chan='st')
            tk.barrier()

        if stop == 'B1':
            od = dout("dbg_h", [TOKB, D])
            tk.dma('sp', 'dbg', od[:, :], hscr_d[:, :])
            od2 = dout("dbg_hn", [D, TOKB], BF16)
            tk.dma('sp', 'dbg', od2[:, :], hnscr_d[:, :])
            tk._wait('sp', {k: v for k, v in tk.dcnt.items()})
            return nc, dbg
        with ExitStack() as pc_:
            wq = sb(pc_, "wq", [128, 16, D], BF16)
            skT = sb(pc_, "skT", [128, 16, 128], BF16)
            with ExitStack() as tmpes:
                wst = [sb(tmpes, f"wqst{i}", [128, D]) for i in range(2)]
                for cc in range(16):
                    DMA(wst[cc % 2][:], wq_d[cc * 128:(cc + 1) * 128, :])
                    CP(wq[:, cc, :], wst[cc % 2][:], eng=('act' if cc % 2 == 0 else 'pool'))
                DMA(wst[0][:], sk_d[:, :])
                CP(skT[:].rearrange("p a b -> p (a b)"), wst[0][:])
                tk.barrier()
            hT4 = [sb(pc_, f"hT4{i}", [128, 16, 512], BF16) for i in range(2)]
            qTs4 = sb(pc_, "qTs4", [128, 16, 512], BF16)
            sc = [sb(pc_, f"sc{i}", [128, 16, 128]) for i in range(2)]
            scr = [sb(pc_, f"scr{i}", [128, 128]) for i in range(2)]
            v12 = sb(pc_, "v12", [128, 16, 16])
            cand = [sb(pc_, f"cand{i}", [128, 16, 16]) for i in range(2)]
            scr2 = [sb(pc_, f"scr2{i}", [128, 256]) for i in range(2)]
            c8 = [sb(pc_, f"c8{i}", [128, 24]) for i in range(2)]
            thr = [sb(pc_, f"thr{i}", [128, 1]) for i in range(2)]
            nm = [sb(pc_, f"nm{i}", [128, 1]) for i in range(2)]
            ej = [sb(pc_, f"ej{i}", [128, 16]) for i in range(2)]
            Zs = [sb(pc_, f"Zs{i}", [128, 1]) for i in range(2)]
            rZ = [sb(pc_, f"rZ{i}", [128, 1]) for i in range(2)]
            nm1 = [sb(pc_, f"nm1{i}", [128, 1]) for i in range(2)]
            nm2 = [sb(pc_, f"nm2{i}", [128, 1]) for i in range(2)]
            E1 = [sb(pc_, f"E1{i}", [128, 128]) for i in range(2)]
            E2 = [sb(pc_, f"E2{i}", [128, 128]) for i in range(2)]
            ga = [sb(pc_, f"ga{i}", [128, 8, 128]) for i in range(2)]
            gp = [sb(pc_, f"gp{i}", [128, 8, 128], BF16) for i in range(2)]
            gc_ = [sb(pc_, f"gc{i}", [128, 8, 128]) for i in range(2)]
            pq = [ps(pc_, f"pq{i}", [128, 512]) for i in range(4)]
            psc = [ps(pc_, f"psc{i}", [128, 4, 128]) for i in range(4)]

            def bc_last(v):
                return bass.AP(v.tensor, v.offset, [list(v.ap[0]), [1, 16], [0, 16]])

            def bc_mid(v):
                return bass.AP(v.tensor, v.offset, [list(v.ap[0]), [0, 16], [1, 16]])

            def interleave(*lists):
                n = max(len(l) for l in lists)
                for k in range(n):
                    for l in lists:
                        if k < len(l):
                            l[k]()

            for j in range(16):
                if j % 4 == 0:
                    hT = hT4[(j // 4) % 2]
                    DMA(hT[:], hnscr_d.ap().rearrange("(c p) t -> p c t", p=128)[:, :, j * 128:(j + 4) * 128])
                    for blk in range(16):
                        MM(pq[blk % 4][:], [(wq[:, dc, blk * 128:(blk + 1) * 128], hT[:, dc, :]) for dc in range(16)])
                        CP(qTs4[:, blk, :], pq[blk % 4][:], eng=('act' if blk % 2 == 0 else 'dve'))
                j4 = j % 4
                scj = sc[j % 2]
                for q4 in range(4):
                    for bi in range(4):
                        blk = q4 * 4 + bi
                        MM(psc[q4][:, bi, :], [(qTs4[:, blk, j4 * 128:(j4 + 1) * 128], skT[:, blk, :])])
                    CP(scj[:, q4 * 4:(q4 + 1) * 4, :], psc[q4][:], eng='act')

                def top16_ops(blk, sr):
                    return [
                        lambda: tk.op('dve', lambda e: e.max(out=v12[:, blk, 0:8], in_=scj[:, blk, :]), reads=[scj], writes=[v12]),
                        lambda: tk.op('dve', lambda e: e.match_replace(out=sr[:], in_to_replace=v12[:, blk, 0:8],
                                                                       in_values=scj[:, blk, :], imm_value=NEG),
                                      reads=[scj, v12], writes=[sr]),
                        lambda: tk.op('dve', lambda e: e.max(out=v12[:, blk, 8:16], in_=sr[:]), reads=[sr], writes=[v12]),
                    ]
                for blk in range(0, 16, 2):
                    interleave(top16_ops(blk, scr[0]), top16_ops(blk + 1, scr[1]))
                gaj, gpj, gcj = ga[j % 2], gp[j % 2], gc_[j % 2]

                def head_ops(h, q):
                    cd, s2_, c8_, th, nm_, ej_, Z_, rZ_, n1, n2, e1, e2 = (cand[q], scr2[q], c8[q], thr[q], nm[q], ej[q], Zs[q],
                                                                          rZ[q], nm1[q], nm2[q], E1[q], E2[q])
                    cf = cd[:].rearrange("p a b -> p (a b)")
                    return [
                        lambda: TT(cd[:], bc_last(v12[:, h, :]), bc_mid(v12[:, 8 + h, :]), ALU.add),
                        lambda: tk.op('dve', lambda e: e.max(out=c8_[:, 0:8], in_=cf), reads=[cd], writes=[c8_]),
                        lambda: tk.op('dve', lambda e: e.match_replace(out=s2_[:], in_to_replace=c8_[:, 0:8], in_values=cf,
                                                                       imm_value=NEG), reads=[cd, c8_], writes=[s2_]),
                        lambda: tk.op('dve', lambda e: e.max(out=c8_[:, 8:16], in_=s2_[:]), reads=[s2_], writes=[c8_]),
                        lambda: tk.op('dve', lambda e: e.match_replace(out=s2_[:], in_to_replace=c8_[:, 8:16], in_values=s2_[:],
                                                                       imm_value=NEG), reads=[s2_, c8_], writes=[s2_]),
                        lambda: tk.op('dve', lambda e: e.max(out=c8_[:, 16:24], in_=s2_[:]), reads=[s2_], writes=[c8_]),
                        lambda: TT(th[:], c8_[:, 15:16], c8_[:, 16:17], ALU.add),
                        lambda: TS(th[:], th[:], 0.5, ALU.mult),
                        lambda: TS(nm_[:], c8_[:, 0:1], -1.0, ALU.mult),
                        lambda: ACT(ej_[:], c8_[:, 0:16], AF.Exp, bias=nm_[:], accum=Z_[:]),
                        lambda: TS(n1[:], v12[:, h, 0:1], -1.0, ALU.mult),
                        lambda: TS(n2[:], v12[:, 8 + h, 0:1], -1.0, ALU.mult),
                        lambda: RECIP(rZ_[:], Z_[:]),
                        lambda: ACT(e1[:], scj[:, h, :], AF.Exp, bias=n1[:]),
                        lambda: ACT(e2[:], scj[:, 8 + h, :], AF.Exp, bias=n2[:]),
                        lambda: STT(e1[:], scj[:, h, :], v12[:, h, 15:16], e1[:], ALU.is_ge, ALU.mult),
                        lambda: TS(gaj[:, h, :], e1[:], rZ_[:], ALU.mult),
                        lambda: STT(gpj[:, h, :], scj[:, 8 + h, :], v12[:, 8 + h, 15:16], e2[:], ALU.is_ge, ALU.mult),
                        lambda: TS(gcj[:, h, :], scj[:, h, :], -1.0, ALU.mult, th[:], ALU.add),
                    ]
                for h in range(0, 8, 2):
                    interleave(head_ops(h, 0), head_ops(h + 1, 1))
                DMA(gs2_d[j], scj[:, 8:16, :].rearrange("p a b -> p (a b)"), chan='st')
                DMA(gc_d[j], gcj[:].rearrange("p a b -> p (a b)"), chan='st')
                DMA(ga_d[j], gaj[:].rearrange("p a b -> p (a b)"), chan='st')
                DMA(gp_d[j], gpj[:].rearrange("p a b -> p (a b)"), chan='st')
                if debug and j == 0:
                    od = dout("dbg_sc", [128, 2048])
                    DMA(od[:, :], scj[:].rearrange("p a b -> p (a b)"), chan='dbg')
            tk.barrier()

        if stop == 'B2':
            for nm_, t_, dt_ in (("dbg_gs2", gs2_d, F32), ("dbg_gc", gc_d, F32), ("dbg_ga", ga_d, F32), ("dbg_gp", gp_d, BF16)):
                od = dout(nm_, [16, 128, 1024], dt_)
                tk.dma('sp', 'dbg', od[:, :, :], t_[:, :, :])
            tk._wait('sp', {k: v for k, v in tk.dcnt.items()})
            return nc, dbg
        with ExitStack() as pd_:
            fing = sb(pd_, "fing", [128, D])
            DMA(fing[:], fin_d[:, :])
            acc = sb(pd_, "acc", [128, 4, D])
            hnT = sb(pd_, "hnT", [128, 16, 512], BF16)
            s2t = sb(pd_, "s2t", [128, 4, 1024])
            cct = sb(pd_, "cct", [128, 4, 1024])
            gat = sb(pd_, "gat", [128, 4, 1024])
            gpt = sb(pd_, "gpt", [128, 4, 1024], BF16)
            ust = [sb(pd_, f"ust{i}", [128, D]) for i in range(2)]
            UT = sb(pd_, "UT", [128, 16, 512], BF16)
            vst = sb(pd_, "vst", [128, D])
            Vb = [sb(pd_, f"Vb{i}", [128, 4, D], BF16) for i in range(2)]
            hg = [sb(pd_, f"hg{i}", [128, 512], BF16) for i in range(3)]
            tmpg = [sb(pd_, f"tmpg{i}", [128, 512], BF16) for i in range(8)]
            Am = [sb(pd_, f"Am{i}", [128, 512], BF16) for i in range(2)]
            AT = [sb(pd_, f"AT{i}", [128, 4, 128], BF16) for i in range(2)]
            ss1 = sb(pd_, "ss1d", [128, 1])
            rs1 = sb(pd_, "rs1d", [128, 1])
            tm1 = sb(pd_, "tm1d", [128, 1])
            osb = ust[0]
            phid = [ps(pd_, f"phid{i}", [128, 512]) for i in range(2)]
            py = [ps(pd_, f"py{i}", [128, 512]) for i in range(2)]
            pG = [ps(pd_, f"pG{i}", [128, 512]) for i in range(2)]
            put = ps(pd_, "put", [128, 4, 128])
            pat = ps(pd_, "pat", [128, 4, 128], BF16)
            NTG = 4
            chunk_box = [0]
            kk_box = [0]
            for tg in range(NTG):
                for j in range(4):
                    jj = tg * 4 + j
                    DMA(acc[:, j, :], hscr_d[jj * 128:(jj + 1) * 128, :])
                    DMA(s2t[:, j, :], gs2_d[jj])
                    DMA(cct[:, j, :], gc_d[jj])
                    DMA(gat[:, j, :], ga_d[jj])
                    DMA(gpt[:, j, :], gp_d[jj])
                DMA(hnT[:], hnscr_d.ap().rearrange("(c p) t -> p c t", p=128)[:, :, tg * 512:(tg + 1) * 512])
                NW = 128

                def load_eg(eg):
                    if tg > 0:
                        DMA(UT[:].rearrange("p a b -> p (a b)"), utscr_d[eg], chan='ldu')
                        DMA(Vb[eg % 2][:].rearrange("p a b -> p (a b)"), vscr_d[eg], chan='ldv')
                        return
                    for ic in range(4):
                        i = eg * 4 + ic
                        u_ = ust[chunk_box[0] % 2]
                        chunk_box[0] += 1
                        DMA(u_[:], pu_d[i * 128:(i + 1) * 128, :], chan='ldu')
                        DMA(vst[:], pv_d[i * 128:(i + 1) * 128, :], chan='ldv')
                        for d4 in range(4):
                            TRS([(put[:, k, :], u_[:, (d4 * 4 + k) * 128:(d4 * 4 + k + 1) * 128], ident) for k in range(4)])
                            CP(UT[:, d4 * 4:(d4 + 1) * 4, ic * 128:(ic + 1) * 128], put[:], eng='act')
                        CP(Vb[eg % 2][:, ic, :], vst[:], eng='pool')
                    DMA(utscr_d[eg], UT[:].rearrange("p a b -> p (a b)"), chan='st')
                    DMA(vscr_d[eg], Vb[eg % 2][:].rearrange("p a b -> p (a b)"), chan='st')

                def hid(w):
                    eg, j = divmod(w, 4)
                    ph = phid[w % 2]
                    MM(ph[:], [(hnT[:, dc, j * 128:(j + 1) * 128], UT[:, dc, :]) for dc in range(16)])
                    ACT(hg[w % 3][:], ph[:], AF.Gelu)

                def gate(w, hq):
                    eg, j = divmod(w, 4)
                    G = pG[w % 2]
                    for h in (2 * hq, 2 * hq + 1):
                        tb = tmpg[kk_box[0] % 8]
                        kk_box[0] += 1
                        for ic in range(4):
                            i = eg * 4 + ic
                            STT(tb[:, ic * 128:(ic + 1) * 128], s2t[:, j, h * 128:(h + 1) * 128],
                                cct[:, j, h * 128 + i:h * 128 + i + 1], gpt[:, j, h * 128:(h + 1) * 128], ALU.is_ge, ALU.mult)
                        for ic in range(4):
                            i = eg * 4 + ic
                            asc = gat[:, j, h * 128 + i:h * 128 + i + 1]
                            if ic % 2 == 0:
                                TS(tb[:, ic * 128:(ic + 1) * 128], tb[:, ic * 128:(ic + 1) * 128], asc, ALU.mult, 1.0, ALU.mult,
                                   eng='pool')
                            else:
                                ACT(tb[:, ic * 128:(ic + 1) * 128], tb[:, ic * 128:(ic + 1) * 128], AF.Copy, scale=asc)
                        MM(G[:], [(identb, tb[:])], first=(h == 0), last=(h == 7))

                def ttA(w):
                    A = Am[w % 2]
                    TT(A[:], hg[w % 3][:], pG[w % 2][:], ALU.mult)
                    TRS([(pat[:, ic, :], A[:, ic * 128:(ic + 1) * 128], identb) for ic in range(4)])
                    CP(AT[w % 2][:], pat[:], eng='act')

                def ymm(w, half):
                    eg, j = divmod(w, 4)
                    for n2 in range(2):
                        n = half * 2 + n2
                        MM(py[n2][:], [(AT[w % 2][:, ic, :], Vb[eg % 2][:, ic, n * 512:(n + 1) * 512]) for ic in range(4)])

                def flush(w, half):
                    j = w % 4
                    for n2 in range(2):
                        n = half * 2 + n2
                        TT(acc[:, j, n * 512:(n + 1) * 512], acc[:, j, n * 512:(n + 1) * 512], py[n2][:], ALU.add)

                load_eg(0)
                hid(0)
                for w in range(NW + 2):
                    if w + 1 < NW:
                        if (w + 1) % 4 == 0:
                            load_eg((w + 1) // 4)
                        hid(w + 1)
                    if w < NW:
                        gate(w, 0)
                    if 0 <= w - 2 < NW:
                        flush(w - 2, 0)
                        ymm(w - 2, 1)
                    if w < NW:
                        gate(w, 1)
                    if 0 <= w - 1 < NW:
                        ttA(w - 1)
                    if w < NW:
                        gate(w, 2)
                    if 0 <= w - 2 < NW:
                        flush(w - 2, 1)
                    if w < NW:
                        gate(w, 3)
                    if 0 <= w - 1 < NW:
                        ymm(w - 1, 0)
                for j in range(4):
                    jj = tg * 4 + j
                    ACT(osb[:], acc[:, j, :], AF.Square, accum=ss1[:])
                    rstd_from_ss(rs1[:], ss1[:], float(D), tm1[:])
                    STT(osb[:], acc[:, j, :], rs1[:], fing[:], ALU.mult, ALU.mult)
                    DMA(out_d[jj * 128:(jj + 1) * 128, :], osb[:], chan='out')
            tk.barrier()
        tk._wait('sp', {k: v for k, v in tk.dcnt.items()})
        print("instructions emitted:", tk.ninst)
    return nc, dbg


def _consts():
    c = np.zeros((128, 4, 128), np.float32)
    s = np.arange(128)[:, None]
    t = np.arange(128)[None, :]
    c[:, 0] = (s == t)
    c[:, 1] = (s <= t)
    c[:, 2] = (s < t)
    c[:, 3] = 1.0
    return c.reshape(128, 512)


def make_in_maps(x, attn_norm_g, w_in, hgrn_lb_logits, hgrn_norm_g, gdn_conv_w, gdn_A_log, gdn_dt_bias, gdn_norm_g,
                 w_out, ffn_norm_g, peer_w_query, peer_sub_keys, peer_u, peer_v, final_norm_g):
    f = lambda a: np.ascontiguousarray(np.asarray(a, dtype=np.float32))
    x = f(x)
    w_in0 = f(w_in)[0]
    lbl = f(hgrn_lb_logits)
    cwf = f(gdn_conv_w)[0]
    wo0 = f(w_out)[0]
    wq0 = f(peer_w_query)[0]
    sk0 = f(peer_sub_keys)[0]
    pu = f(peer_u)[0]
    pv = f(peer_v)[0]
    chunked = lambda v: np.ascontiguousarray(v.reshape(16, 128).T)
    ag = chunked(f(attn_norm_g)[0])
    fg = chunked(f(ffn_norm_g)[0])
    fin = np.ascontiguousarray(np.broadcast_to(f(final_norm_g)[None, :], (128, D)))
    hg = f(hgrn_norm_g)[0]
    gg = f(gdn_norm_g)[0]
    gain512 = np.ascontiguousarray(np.broadcast_to(np.concatenate([hg, hg, gg, gg])[None, :], (128, 512)))
    wq = np.ascontiguousarray(wq0.reshape(D, 8, 2, 128).transpose(0, 2, 1, 3).reshape(D, D))
    skt = np.ascontiguousarray(sk0.transpose(3, 0, 1, 2).reshape(128, 16 * 128))
    consts = _consts()
    maps = []
    for c in range(8):
        b, g = c // 4, c % 4
        cols = np.concatenate([
            np.arange(0 + 2 * g * 128, 0 + 2 * g * 128 + 256),
            np.arange(1024 + 2 * g * 128, 1024 + 2 * g * 128 + 256),
            np.arange(4096 + g * 128, 4096 + g * 128 + 128),
            np.arange(4608 + g * 128, 4608 + g * 128 + 128),
            np.arange(5120 + 2 * g * 128, 5120 + 2 * g * 128 + 256),
            np.arange(2048 + 2 * g * 128, 2048 + 2 * g * 128 + 256),
            np.arange(3072 + 2 * g * 128, 3072 + 2 * g * 128 + 256),
            np.arange(6144 + 2 * g * 128, 6144 + 2 * g * 128 + 256),
            np.arange(7168 + 2 * g, 7168 + 2 * g + 2),
            np.arange(7176 + 2 * g, 7176 + 2 * g + 2),
        ])
        wloc = np.ascontiguousarray(w_in0[:, cols])
        lb4 = np.ascontiguousarray(lbl[:, 2 * g:2 * g + 2, :].transpose(2, 0, 1).reshape(128, 4))
        cch = np.concatenate([np.arange(g * 128, g * 128 + 128), np.arange(512 + g * 128, 512 + g * 128 + 128),
                              np.arange(1024 + 2 * g * 128, 1024 + 2 * g * 128 + 256)])
        cwl = np.ascontiguousarray(cwf[:, cch].reshape(4, 4, 128).transpose(2, 1, 0).reshape(128, 16))
        al = np.ascontiguousarray(np.broadcast_to(f(gdn_A_log)[0, 2 * g:2 * g + 2][None, :], (128, 2)))
        dtb = np.ascontiguousarray(np.broadcast_to(f(gdn_dt_bias)[0, 2 * g:2 * g + 2][None, :], (128, 2)))
        maps.append({
            "x": x[b], "xs": np.ascontiguousarray(x[b, g * TOKB:(g + 1) * TOKB]),
            "w_in": wloc, "attn_g": ag, "lb_logits": lb4, "gain512": gain512, "conv_w": cwl,
            "a_log": al, "dt_bias": dtb, "w_out": None, "ffn_g": fg, "w_query": wq, "sk_t": skt,
            "peer_u": pu, "peer_v": pv, "final_g": fin, "consts": consts,
            "tok_off": np.array([[g * 8192]], np.int32),
        })
    rows = []
    for r in range(4):
        for q in range(4):
            base = (2 * r + q) * 128 if q < 2 else 1024 + (2 * r + q - 2) * 128
            rows.append(np.arange(base, base + 128))
    wop = np.ascontiguousarray(wo0[np.concatenate(rows), :])
    for m in maps:
        m["w_out"] = wop
    return maps


_CACHE = {}


def kernel(**inputs):
    if "nc" not in _CACHE:
        _CACHE["nc"] = build_program(debug=False)[0]
    nc = _CACHE["nc"]
    maps = make_in_maps(**inputs)
    res = run_bass_kernel_spmd(nc, maps, core_ids=list(range(8)))
    out = np.zeros((2, SEQ, D), np.float32)
    for c in range(8):
        b, g = c // 4, c % 4
        out[b, g * TOKB:(g + 1) * TOKB] = res.results[c]["out"]
    return out
```
